# Optimizing a Trainium2 kernel written in Bass

```python
import jax, jax.numpy as jnp
from jax import lax
import numpy as np

D_MODEL = 1024
BATCH = 4
SEQ = 8192
DEPTH = 4

CHUNK = 64
Q_BLOCK = 128
N_MIXERS = 2
EPS = 1e-6

MLA_HEADS = 16
MLA_NOPE = 64
MLA_ROPE = 32
MLA_V = 64
MLA_Q_LORA = 384
MLA_KV_LORA = 256
MLA_IN = MLA_Q_LORA + MLA_KV_LORA + MLA_ROPE
ROPE_THETA = 10000.0

SSM_EXPAND = 2
D_INNER = SSM_EXPAND * D_MODEL
SSM_HEADDIM = 64
SSM_HEADS = D_INNER // SSM_HEADDIM
SSM_GROUPS = 4
SSM_HPG = SSM_HEADS // SSM_GROUPS
SSM_STATE = 128
SSM_CONV = 4
SSM_CHUNK = CHUNK
CONV_DIM = D_INNER + 2 * SSM_GROUPS * SSM_STATE
SSM_IN = D_INNER + CONV_DIM + SSM_HEADS

D_FF = 2816
N_EXPERTS = 8
TOP_K = 2
D_EXPERT = 2816

kernel_name = "hybrid_mla_mamba2_moe_adaln_stream"


def rmsnorm(x, g):
    xf = x.astype(jnp.float32)
    y = xf * lax.rsqrt(jnp.mean(xf * xf, axis=-1, keepdims=True) + EPS)
    return (y * g.astype(jnp.float32)).astype(x.dtype)


def modulate(h, shift, scale):
    return h * (1.0 + scale[:, None, :]) + shift[:, None, :]


def rope_tables(positions):
    inv_freq = ROPE_THETA ** (-jnp.arange(0, MLA_ROPE, 2, dtype=jnp.float32) / MLA_ROPE)
    ang = positions.astype(jnp.float32)[..., None] * inv_freq
    return jnp.cos(ang), jnp.sin(ang)


def apply_rope(t, cos, sin):
    t1, t2 = jnp.split(t, 2, axis=-1)
    cos = cos.astype(t.dtype)
    sin = sin.astype(t.dtype)
    return jnp.concatenate([t1 * cos - t2 * sin, t1 * sin + t2 * cos], axis=-1)


def chunk_causal_mla_attention(q_nope, q_rope, k_nope, k_rope, v):
    bsz, seq, nh, _ = q_nope.shape
    nb = seq // Q_BLOCK
    scale = (MLA_NOPE + MLA_ROPE) ** -0.5
    qn = q_nope.reshape(bsz, nb, Q_BLOCK, nh, MLA_NOPE).transpose(1, 0, 2, 3, 4)
    qr = q_rope.reshape(bsz, nb, Q_BLOCK, nh, MLA_ROPE).transpose(1, 0, 2, 3, 4)
    key_chunk = jnp.arange(seq) // CHUNK

    def one_block(args):
        blk, qn_b, qr_b = args
        s = (jnp.einsum('bqhd,bkhd->bhqk', qn_b, k_nope, preferred_element_type=jnp.float32)
             + jnp.einsum('bqhr,bkr->bhqk', qr_b, k_rope, preferred_element_type=jnp.float32)) * scale
        q_chunk = (blk * Q_BLOCK + jnp.arange(Q_BLOCK)) // CHUNK
        mask = key_chunk[None, :] <= q_chunk[:, None]
        s = jnp.where(mask[None, None], s, -jnp.inf)
        p = jax.nn.softmax(s, axis=-1).astype(v.dtype)
        return jnp.einsum('bhqk,bkhd->bqhd', p, v)

    o = lax.map(one_block, (jnp.arange(nb), qn, qr))
    return o.transpose(1, 0, 2, 3, 4).reshape(bsz, seq, nh, MLA_V)


def mla_mixer(h, cos, sin, w_in, q_norm, kv_norm, w_uq, w_ukv, w_out):
    bsz, seq, _ = h.shape
    proj = h @ w_in
    c_q = rmsnorm(proj[..., :MLA_Q_LORA], q_norm)
    c_kv = rmsnorm(proj[..., MLA_Q_LORA:MLA_Q_LORA + MLA_KV_LORA], kv_norm)
    k_rope = apply_rope(proj[..., MLA_Q_LORA + MLA_KV_LORA:], cos, sin)
    q = (c_q @ w_uq).reshape(bsz, seq, MLA_HEADS, MLA_NOPE + MLA_ROPE)
    q_nope = q[..., :MLA_NOPE]
    q_rope = apply_rope(q[..., MLA_NOPE:], cos[:, :, None, :], sin[:, :, None, :])
    kv = (c_kv @ w_ukv).reshape(bsz, seq, MLA_HEADS, MLA_NOPE + MLA_V)
    k_nope = kv[..., :MLA_NOPE]
    v = kv[..., MLA_NOPE:]
    o = chunk_causal_mla_attention(q_nope, q_rope, k_nope, k_rope, v)
    return o.reshape(bsz, seq, MLA_HEADS * MLA_V) @ w_out


def causal_depthwise_conv(u, w, b):
    y = lax.conv_general_dilated(
        u, w[:, None, :].astype(u.dtype), window_strides=(1,), padding=[(SSM_CONV - 1, 0)],
        dimension_numbers=('NWC', 'WIO', 'NWC'), feature_group_count=u.shape[-1])
    return y + b.astype(u.dtype)


def ssd_chunked(x, dt, a, bm, cm):
    bsz, seq = x.shape[:2]
    nc = seq // SSM_CHUNK
    L = SSM_CHUNK
    x = x.reshape(bsz, nc, L, SSM_GROUPS, SSM_HPG, SSM_HEADDIM)
    dt = dt.reshape(bsz, nc, L, SSM_GROUPS, SSM_HPG)
    bm = bm.reshape(bsz, nc, L, SSM_GROUPS, SSM_STATE)
    cm = cm.reshape(bsz, nc, L, SSM_GROUPS, SSM_STATE)
    a_cs = jnp.cumsum(dt * a.reshape(SSM_GROUPS, SSM_HPG), axis=2)
    xdt = x * dt[..., None]
    a_t = a_cs.transpose(0, 1, 3, 4, 2)
    seg = a_t[..., :, None] - a_t[..., None, :]
    causal = jnp.tril(jnp.ones((L, L), dtype=bool))
    decay = jnp.where(causal, jnp.exp(jnp.where(causal, seg, 0.0)), 0.0)
    cb = jnp.einsum('bclgn,bcsgn->bcgls', cm, bm)
    y_diag = jnp.einsum('bcgls,bcghls,bcsghp->bclghp', cb, decay, xdt)
    decay_to_end = jnp.exp(a_cs[:, :, -1:] - a_cs)
    states = jnp.einsum('bclgn,bclgh,bclghp->bcghpn', bm, decay_to_end, xdt)
    chunk_decay = jnp.exp(a_cs[:, :, -1])

    def step(state, inp):
        st_c, dec_c = inp
        return state * dec_c[..., None, None] + st_c, state

    init = jnp.zeros((bsz, SSM_GROUPS, SSM_HPG, SSM_HEADDIM, SSM_STATE), jnp.float32)
    _, prev = lax.scan(step, init, (jnp.moveaxis(states, 1, 0), jnp.moveaxis(chunk_decay, 1, 0)))
    prev = jnp.moveaxis(prev, 0, 1)
    y_off = jnp.einsum('bclgn,bcghpn,bclgh->bclghp', cm, prev, jnp.exp(a_cs))
    return (y_diag + y_off).reshape(bsz, seq, SSM_HEADS, SSM_HEADDIM)


def mamba2_mixer(h, w_in, conv_w, conv_b, dt_bias, a_log, d_skip, norm_g, w_out):
    bsz, seq, _ = h.shape
    proj = h @ w_in
    z = proj[..., :D_INNER]
    xbc = jax.nn.silu(causal_depthwise_conv(proj[..., D_INNER:D_INNER + CONV_DIM], conv_w, conv_b))
    dt_raw = proj[..., D_INNER + CONV_DIM:]
    xs = xbc[..., :D_INNER].reshape(bsz, seq, SSM_HEADS, SSM_HEADDIM).astype(jnp.float32)
    bm = xbc[..., D_INNER:D_INNER + SSM_GROUPS * SSM_STATE].reshape(bsz, seq, SSM_GROUPS, SSM_STATE).astype(jnp.float32)
    cm = xbc[..., D_INNER + SSM_GROUPS * SSM_STATE:].reshape(bsz, seq, SSM_GROUPS, SSM_STATE).astype(jnp.float32)
    dt = jax.nn.softplus(dt_raw.astype(jnp.float32) + dt_bias.astype(jnp.float32))
    a = -jnp.exp(a_log.astype(jnp.float32))
    y = ssd_chunked(xs, dt, a, bm, cm) + d_skip.astype(jnp.float32)[:, None] * xs
    y = y.reshape(bsz, seq, D_INNER).astype(h.dtype)
    yg = (y * jax.nn.silu(z)).reshape(bsz, seq, SSM_GROUPS, D_INNER // SSM_GROUPS)
    yf = yg.astype(jnp.float32)
    yf = yf * lax.rsqrt(jnp.mean(yf * yf, axis=-1, keepdims=True) + EPS)
    y = (yf.reshape(bsz, seq, D_INNER) * norm_g.astype(jnp.float32)).astype(h.dtype)
    return y @ w_out


def swiglu(t, w_gate, w_up, w_down):
    return (jax.nn.silu(t @ w_gate) * (t @ w_up)) @ w_down


def moe_swiglu(h, w_router, w_gate, w_up, w_down):
    bsz, seq, d = h.shape
    t = h.reshape(bsz * seq, d)
    logits = (t @ w_router).astype(jnp.float32)
    top_vals, top_idx = lax.top_k(logits, TOP_K)
    top_w = jax.nn.softmax(top_vals, axis=-1)
    gates = jnp.sum(jax.nn.one_hot(top_idx, N_EXPERTS, dtype=jnp.float32) * top_w[..., None], axis=1)
    out = jnp.zeros_like(t)
    for e in range(N_EXPERTS):
        out = out + gates[:, e:e + 1].astype(t.dtype) * swiglu(t, w_gate[e], w_up[e], w_down[e])
    return out.reshape(bsz, seq, d)


def setup_inputs(seed: int = 0) -> dict:
    key = jax.random.key(seed)
    ks = jax.random.split(key, 40)
    n_att = (DEPTH + 1) // 2
    n_ssm = DEPTH // 2
    f32 = jnp.float32

    def nrm(i, shape, fan_in, mult=1.0):
        return jax.random.normal(ks[i], shape, f32) * (mult * fan_in ** -0.5)

    def gain(i, shape):
        return 1.0 + 0.1 * jax.random.normal(ks[i], shape, f32)

    x = jax.random.normal(ks[0], (BATCH, SEQ, D_MODEL), f32)
    c = jax.random.normal(ks[1], (BATCH, D_MODEL), f32)
    offsets = jax.random.randint(ks[2], (BATCH, 1), 0, 4096, dtype=jnp.int32)
    positions = (jnp.arange(SEQ, dtype=jnp.int32)[None, :] + offsets).astype(jnp.int32)
    dt0 = jnp.exp(jax.random.uniform(ks[3], (n_ssm, SSM_HEADS), f32, np.log(1e-3), np.log(1e-1)))
    return {
        "x": x,
        "c": c,
        "positions": positions,
        "ada_w": nrm(4, (DEPTH, D_MODEL, 6 * D_MODEL), D_MODEL, 0.5),
        "ada_b": 0.02 * jax.random.normal(ks[5], (DEPTH, 6 * D_MODEL), f32),
        "norm_g": gain(6, (DEPTH, 2, D_MODEL)),
        "mla_w_in": nrm(7, (n_att, D_MODEL, MLA_IN), D_MODEL),
        "mla_q_norm": gain(8, (n_att, MLA_Q_LORA)),
        "mla_kv_norm": gain(9, (n_att, MLA_KV_LORA)),
        "mla_w_uq": nrm(10, (n_att, MLA_Q_LORA, MLA_HEADS * (MLA_NOPE + MLA_ROPE)), MLA_Q_LORA),
        "mla_w_ukv": nrm(11, (n_att, MLA_KV_LORA, MLA_HEADS * (MLA_NOPE + MLA_V)), MLA_KV_LORA),
        "mla_w_out": nrm(12, (n_att, MLA_HEADS * MLA_V, D_MODEL), MLA_HEADS * MLA_V),
        "ssm_w_in": nrm(13, (n_ssm, D_MODEL, SSM_IN), D_MODEL),
        "ssm_conv_w": nrm(14, (n_ssm, SSM_CONV, CONV_DIM), SSM_CONV),
        "ssm_conv_b": 0.02 * jax.random.normal(ks[15], (n_ssm, CONV_DIM), f32),
        "ssm_dt_bias": dt0 + jnp.log(-jnp.expm1(-dt0)),
        "ssm_a_log": jnp.log(jax.random.uniform(ks[16], (n_ssm, SSM_HEADS), f32, 1.0, 16.0)),
        "ssm_d": gain(17, (n_ssm, SSM_HEADS)),
        "ssm_norm": gain(18, (n_ssm, D_INNER)),
        "ssm_w_out": nrm(19, (n_ssm, D_INNER, D_MODEL), D_INNER),
        "ffn_w_gate": nrm(20, (n_att, D_MODEL, D_FF), D_MODEL),
        "ffn_w_up": nrm(21, (n_att, D_MODEL, D_FF), D_MODEL),
        "ffn_w_down": nrm(22, (n_att, D_FF, D_MODEL), D_FF),
        "moe_w_router": nrm(23, (n_ssm, D_MODEL, N_EXPERTS), D_MODEL),
        "moe_w_gate": nrm(24, (n_ssm, N_EXPERTS, D_MODEL, D_EXPERT), D_MODEL),
        "moe_w_up": nrm(25, (n_ssm, N_EXPERTS, D_MODEL, D_EXPERT), D_MODEL),
        "moe_w_down": nrm(26, (n_ssm, N_EXPERTS, D_EXPERT, D_MODEL), D_EXPERT),
        "final_norm": gain(27, (D_MODEL,)),
    }


def reference(x, c, positions, ada_w, ada_b, norm_g,
              mla_w_in, mla_q_norm, mla_kv_norm, mla_w_uq, mla_w_ukv, mla_w_out,
              ssm_w_in, ssm_conv_w, ssm_conv_b, ssm_dt_bias, ssm_a_log, ssm_d, ssm_norm, ssm_w_out,
              ffn_w_gate, ffn_w_up, ffn_w_down,
              moe_w_router, moe_w_gate, moe_w_up, moe_w_down, final_norm):
    cos, sin = rope_tables(positions)
    cond = jax.nn.silu(c)
    for i in range(DEPTH):
        j = i // N_MIXERS
        mod = cond @ ada_w[i] + ada_b[i]
        sh1, sc1, g1, sh2, sc2, g2 = jnp.split(mod, 6, axis=-1)
        h = modulate(rmsnorm(x, norm_g[i, 0]), sh1, sc1)
        if i % N_MIXERS == 0:
            y = mla_mixer(h, cos, sin, mla_w_in[j], mla_q_norm[j], mla_kv_norm[j],
                          mla_w_uq[j], mla_w_ukv[j], mla_w_out[j])
        else:
            y = mamba2_mixer(h, ssm_w_in[j], ssm_conv_w[j], ssm_conv_b[j], ssm_dt_bias[j],
                             ssm_a_log[j], ssm_d[j], ssm_norm[j], ssm_w_out[j])
        x = x + g1[:, None, :] * y
        h = modulate(rmsnorm(x, norm_g[i, 1]), sh2, sc2)
        if i % 2 == 0:
            y = swiglu(h, ffn_w_gate[j], ffn_w_up[j], ffn_w_down[j])
        else:
            y = moe_swiglu(h, moe_w_router[j], moe_w_gate[j], moe_w_up[j], moe_w_down[j])
        x = x + g2[:, None, :] * y
    return rmsnorm(x, final_norm)
```

```python
import contextlib
import numpy as np
import concourse.bass as bass
import concourse.mybir as mybir
from concourse.bass_utils import run_bass_kernel_spmd

F32 = mybir.dt.float32
BF16 = mybir.dt.bfloat16
I32 = mybir.dt.int32
AF = mybir.ActivationFunctionType
ALU = mybir.AluOpType
AX = mybir.AxisListType

D = 1024
NCORES = 8
EPS = 1e-6

ENGS = ("pe", "act", "dve", "pool", "sp")


class _Op:
    __slots__ = ("eng", "fn", "is_dma", "is_mm", "waits", "need_inc", "count", "dsem", "dval")

    def __init__(self, eng, fn, is_dma, is_mm):
        self.eng = eng
        self.fn = fn
        self.is_dma = is_dma
        self.is_mm = is_mm
        self.waits = []
        self.need_inc = False
        self.count = 0
        self.dsem = None
        self.dval = 0


class Sched:
    def __init__(self, nc, dma_slots=8):
        self.nc = nc
        self.dma_slots = dma_slots
        self.dma_n = {e: 0 for e in ENGS}
        self.cnt = {e: 0 for e in ENGS}
        self.out_dmas = []
        self.st = contextlib.ExitStack()
        self.csem = {e: self.st.enter_context(nc.semaphore(f"c_{e}")) for e in ENGS}
        self.dsem = {}
        for e in ("sp", "act", "pool"):
            for s_ in range(dma_slots):
                self.dsem[(e, s_)] = self.st.enter_context(nc.semaphore(f"d_{e}{s_}"))
        self.barrier = {}
        self.seen = {e: {} for e in ENGS}
        self._reset()

    def _reset(self):
        self.ops = {e: [] for e in ENGS}
        self.last_w = {}
        self.readers = {}

    def add(self, eng, fn, R=(), W=(), dma=False, mm=False, is_out=False):
        op = _Op(eng, fn, dma, mm)
        deps = []
        for r in R:
            w = self.last_w.get(r)
            if w is not None:
                deps.append(w)
        for w_ in W:
            w = self.last_w.get(w_)
            if w is not None:
                deps.append(w)
            deps.extend(self.readers.get(w_, ()))
        seen = set()
        for d in deps:
            if id(d) in seen or d is op:
                continue
            seen.add(id(d))
            if d.eng == eng and not d.is_dma and not dma and eng == "pe" and d.is_mm and mm:
                continue
            op.waits.append(d)
        for r in R:
            self.readers.setdefault(r, []).append(op)
        for w_ in W:
            self.last_w[w_] = op
            self.readers[w_] = []
        self.ops[eng].append(op)
        if dma:
            n = self.dma_n[eng]
            self.dma_n[eng] = n + 1
            op.dsem = (eng, n % self.dma_slots)
            op.dval = 16 * (n // self.dma_slots + 1)
            if is_out:
                self.out_dmas.append(op)
        return op

    def emit_phase(self, last=False):
        nc = self.nc
        for e in ENGS:
            for op in self.ops[e]:
                for d in op.waits:
                    if not d.is_dma:
                        d.need_inc = True
            comp = [op for op in self.ops[e] if not op.is_dma]
            if comp:
                comp[-1].need_inc = True
        for e in ENGS:
            c = self.cnt[e]
            for op in self.ops[e]:
                if not op.is_dma and op.need_inc:
                    c += 1
                    op.count = c
            self.cnt[e] = c
        csem, dsem = self.csem, self.dsem
        barrier = dict(self.barrier)
        ops = self.ops
        out_dmas = self.out_dmas
        seen_all = self.seen

        def run(e, eng):
            seen = seen_all[e]
            for key, val in barrier.items():
                if key == ("c", e) or seen.get(key, 0) >= val:
                    continue
                seen[key] = val
                eng.wait_ge(dsem[key[1]] if key[0] == "d" else csem[key[1]], val)
            for op in ops[e]:
                need = {}
                for d in op.waits:
                    if d.is_dma:
                        key = ("d", d.dsem)
                        val = d.dval
                    else:
                        key = ("c", d.eng)
                        val = d.count
                    if need.get(key, 0) < val:
                        need[key] = val
                if op.is_dma and op.dval > 16:
                    key = ("d", op.dsem)
                    if need.get(key, 0) < op.dval - 16:
                        need[key] = op.dval - 16
                for key, val in need.items():
                    if seen.get(key, 0) >= val:
                        continue
                    seen[key] = val
                    eng.wait_ge(dsem[key[1]] if key[0] == "d" else csem[key[1]], val)
                inst = op.fn(eng)
                if op.is_dma:
                    inst.then_inc(dsem[op.dsem], 16)
                elif op.need_inc:
                    inst.then_inc(csem[e], 1)
            if last and e == "sp":
                fin = {}
                for op in out_dmas:
                    if fin.get(op.dsem, 0) < op.dval:
                        fin[op.dsem] = op.dval
                for k, v in fin.items():
                    eng.wait_ge(dsem[k], v)

        with nc.Block() as block:
            @block.tensor
            def _(eng):
                run("pe", eng)

            @block.scalar
            def _(eng):
                run("act", eng)

            @block.vector
            def _(eng):
                run("dve", eng)

            @block.gpsimd
            def _(eng):
                run("pool", eng)

            @block.sync
            def _(eng):
                run("sp", eng)
        for e in ENGS:
            if self.cnt[e]:
                self.barrier[("c", e)] = self.cnt[e]
            n = self.dma_n[e]
            for s_ in range(min(self.dma_slots, n)):
                last_n = n - 1 - ((n - 1 - s_) % self.dma_slots)
                self.barrier[("d", (e, s_))] = 16 * (last_n // self.dma_slots + 1)
        self._reset()

    def close(self):
        self.st.close()


class Prog:
    def __init__(self):
        self.nc = bass.Bass("TRN2", target_bir_lowering=False)
        self.S = Sched(self.nc)
        self.st = contextlib.ExitStack()
        self.outs = []
        self.bind = {}
        self.nphase = 0

    def din(self, name, shape, dt=F32):
        if name in self.bind:
            return self.bind[name]
        return self.nc.dram_tensor(name, list(shape), dt, kind="ExternalInput").ap()

    def dout(self, name, shape, dt=F32):
        if name in self.bind:
            return self.bind[name]
        self.outs.append(name)
        return self.nc.dram_tensor(name, list(shape), dt, kind="ExternalOutput").ap()

    def scratch(self, name, shape, dt=F32):
        return self.nc.dram_tensor(name, list(shape), dt, kind="Internal").ap()

    def sb(self, name, shape, dt=F32):
        return self.st.enter_context(self.nc.sbuf_tensor(f"s{self.nphase}_" + name, list(shape), dt))

    def ps(self, name, shape=(128, 512), dt=F32):
        return self.st.enter_context(self.nc.psum_tensor(f"p{self.nphase}_" + name, list(shape), dt))

    def end_phase(self, last=False):
        self.S.emit_phase(last=last)
        self.st.close()
        self.st = contextlib.ExitStack()
        self.nphase += 1
        self.bind = {}

    def dma(self, q, out, in_, R=(), W=(), is_out=False, slow=False):
        kw = {"allow_slow_non_contiguous": True}
        return self.S.add(q, lambda e: e.dma_start(out=out, in_=in_, **kw), R, W, dma=True, is_out=is_out)

    def mm(self, out, lhsT, rhs, start, stop, R=(), W=()):
        return self.S.add("pe", lambda e: e.matmul(out, lhsT, rhs, start=start, stop=stop), R, W, mm=True)

    def tr(self, out, in_, ident, R=(), W=()):
        return self.S.add("pe", lambda e: e.transpose(out, in_, ident), R, W, mm=True)

    def act(self, out, in_, func, bias=None, scale=None, accum_out=None, R=(), W=()):
        kw = {}
        if bias is not None:
            kw["bias"] = bias
        if scale is not None:
            kw["scale"] = scale
        if accum_out is not None:
            kw["accum_out"] = accum_out
        return self.S.add("act", lambda e: e.activation(out, in_, func, **kw), R, W)

    def ts(self, eng, out, in0, s1, s2, op0, op1=None, R=(), W=()):
        if op1 is None:
            return self.S.add(eng, lambda e: e.tensor_scalar(out, in0, s1, None, op0), R, W)
        return self.S.add(eng, lambda e: e.tensor_scalar(out, in0, s1, s2, op0, op1), R, W)

    def tt(self, eng, out, in0, in1, op, R=(), W=()):
        return self.S.add(eng, lambda e: e.tensor_tensor(out, in0, in1, op), R, W)

    def stt(self, out, in0, scalar, in1, op0, op1, R=(), W=()):
        return self.S.add("dve", lambda e: e.scalar_tensor_tensor(out, in0, scalar, in1, op0, op1), R, W)

    def red(self, out, in_, op, R=(), W=()):
        return self.S.add("dve", lambda e: e.tensor_reduce(out, in_, AX.X, op), R, W)

    def recip(self, out, in_, R=(), W=()):
        return self.S.add("dve", lambda e: e.reciprocal(out, in_), R, W)

    def copy(self, eng, out, in_, R=(), W=()):
        return self.S.add(eng, lambda e: e.tensor_copy(out, in_), R, W)

    def memset(self, eng, ap, val, W=()):
        return self.S.add(eng, lambda e: e.memset(ap, val), (), W)

    def finish(self):
        self.end_phase(last=True)
        self.S.close()
        return self.nc


def emit_norm_tile(p, c, xt, xt_tok, ti, hT, hT_tokbase, h32=None):
    k = c["nt"]
    c["nt"] = k + 1
    b = k % 2
    xn = c["xn"][b]
    ss = c["ss"][b]
    R, Wt = [xt_tok], [f"xn{b}", f"ss{b}"]
    p.act(xn[:, :], xt[:, :], AF.Square, accum_out=ss[:, 0:1], R=R, W=Wt)
    p.ts("dve", ss[:, 1:2], ss[:, 0:1], 1.0 / D, EPS, ALU.mult, ALU.add, R=[f"ss{b}"], W=[f"ss{b}"])
    p.act(ss[:, 2:3], ss[:, 1:2], AF.Sqrt, R=[f"ss{b}"], W=[f"ss{b}"])
    p.recip(ss[:, 3:4], ss[:, 2:3], R=[f"ss{b}"], W=[f"ss{b}"])
    p.ts("dve", xn[:, :], xt[:, :], ss[:, 3:4], None, ALU.mult, R=[xt_tok, f"ss{b}"], W=[f"xn{b}"])
    for half in range(2):
        bank = c["tp"][half]
        btok = c["tp_tok"][half]
        for q in range(4):
            kc = half * 4 + q
            p.tr(bank[:, q * 128:(q + 1) * 128], xn[:, kc * 128:(kc + 1) * 128], c["ident"][:, :],
                 R=[f"xn{b}", "ident"], W=[btok])
        for q in range(4):
            kc = half * 4 + q
            p.act(hT[:, kc, ti * 128:(ti + 1) * 128], bank[:, q * 128:(q + 1) * 128], AF.Identity,
                  bias=c["sh"][:, kc:kc + 1], scale=c["a"][:, kc:kc + 1],
                  R=[btok, "modv"], W=[f"{hT_tokbase}.{ti}.{kc}"])
            if h32 is not None:
                p.act(h32[:, kc, :], bank[:, q * 128:(q + 1) * 128], AF.Identity,
                      bias=c["sh"][:, kc:kc + 1], scale=c["a"][:, kc:kc + 1],
                      R=[btok, "modv"], W=[f"h32.{kc}"])


FF = 2816
NFF = FF // 128
UNITS = [(0, 3), (3, 3), (6, 3), (9, 3), (12, 3), (15, 3), (18, 2), (20, 2)]


def build_ffn(T, E, route, final, dbg=0, **kw):
    p = Prog()
    emit_ffn(p, T, E, route, final, dbg, **kw)
    return p.finish()


def emit_ffn(p, T, E, route, final, dbg=0, **kw):
    x_in = p.din("x", [T, D])
    a_in = p.din("a2", [128, 8])
    sh_in = p.din("sh2", [128, 8])
    g2b_in = p.din("g2b", [128, D])
    wg = p.din("wg", [E, D, FF])
    wu = p.din("wu", [E, D, FF])
    wd = p.din("wd", [E, FF, D])
    ident_in = p.din("ident", [128, 128])
    if route:
        wr_in = p.din("wr", [D, 8])
    if final:
        fg_in = p.din("fgb", [128, D])
    y_out = p.dout("y", [T, D])

    SG = min(T, 2048)
    NSG = T // SG
    NT = SG // 128
    NG = SG // 512

    ident = p.sb("ident", [128, 128])
    a_sb = p.sb("a_sb", [128, 8])
    sh_sb = p.sb("sh_sb", [128, 8])
    g2b = p.sb("g2b_sb", [128, D])
    hT = p.sb("hT", [128, 8, SG], BF16)
    acc = p.sb("acc", [128, NT, D])
    xt = [p.sb(f"xt{i}", [128, D]) for i in range(2)]
    xn = [p.sb(f"xn{i}", [128, D]) for i in range(2)]
    ss = [p.sb(f"ss{i}", [128, 4]) for i in range(2)]
    wgs = [p.sb(f"wg{i}", [128, 8, 384], BF16) for i in range(2)]
    wus = [p.sb(f"wu{i}", [128, 8, 384], BF16) for i in range(2)]
    wds = [p.sb(f"wd{i}", [128, 3, D], BF16) for i in range(2)]
    aT = [p.sb(f"aT{i}", [128, 3, 512], BF16) for i in range(2)]
    sl = [p.sb(f"sl{i}", [128, 512]) for i in range(2)]
    gates = p.sb("gates", [128, NT, 8])
    if route:
        wr = p.sb("wr_sb", [128, 8, 8])
        h32 = p.sb("h32", [128, 8, 128])
        gsc = p.sb("gsc", [128, 48])
    if final:
        fgb = p.sb("fgb_sb", [128, D])
    banks = [p.ps(f"bank{i}") for i in range(8)]

    p.dma("sp", ident[:, :], ident_in, W=["ident"])
    p.dma("sp", a_sb[:, :], a_in, W=["modv"])
    p.dma("sp", sh_sb[:, :], sh_in, W=["modv"])
    p.dma("sp", g2b[:, :], g2b_in, W=["g2b"])
    if route:
        p.dma("sp", wr[:, :, :], wr_in.rearrange("(kc p) e -> p kc e", p=128), W=["wr"])
    if final:
        p.dma("sp", fgb[:, :], fg_in, W=["fgb"])

    c = {"nt": 0, "xn": xn, "ss": ss, "tp": [banks[0], banks[1]], "tp_tok": ["bank0", "bank1"],
         "ident": ident, "a": a_sb, "sh": sh_sb}

    nunit = 0
    nld = 0
    for sg in range(NSG):
        tok0 = sg * SG
        for ti in range(NT):
            b = nld % 2
            nld += 1
            p.dma("sp", xt[b][:, :], x_in[tok0 + ti * 128: tok0 + (ti + 1) * 128, :], W=[f"xt{b}"])
            emit_norm_tile(p, c, xt[b], f"xt{b}", ti, hT, "hT", h32 if route else None)
            if route:
                lg_ps = banks[2]
                for kc in range(8):
                    p.mm(lg_ps[:, 0:8], h32[:, kc, :], wr[:, kc, :], kc == 0, kc == 7,
                         R=[f"h32.{kc}", "wr"], W=["bank2"])
                G = ["gsc"]
                lg = gsc[:, 0:8]
                m1 = gsc[:, 8:9]
                mk1 = gsc[:, 16:24]
                l2 = gsc[:, 24:32]
                m2 = gsc[:, 9:10]
                mk2 = gsc[:, 32:40]
                dd = gsc[:, 10:11]
                ex = gsc[:, 11:12]
                g1 = gsc[:, 12:13]
                g2_ = gsc[:, 13:14]
                gt = gsc[:, 40:48]
                p.copy("dve", lg, lg_ps[:, 0:8], R=["bank2"], W=G)
                p.red(m1, lg, ALU.max, R=G, W=G)
                p.ts("dve", mk1, lg, m1, None, ALU.is_equal, R=G, W=G)
                p.stt(l2, mk1, -1e30, lg, ALU.mult, ALU.add, R=G, W=G)
                p.red(m2, l2, ALU.max, R=G, W=G)
                p.ts("dve", mk2, l2, m2, None, ALU.is_equal, R=G, W=G)
                p.tt("dve", dd, m2, m1, ALU.subtract, R=G, W=G)
                p.act(ex, dd, AF.Exp, R=G, W=G)
                p.ts("dve", g1, ex, 1.0, None, ALU.add, R=G, W=G)
                p.recip(g1, g1, R=G, W=G)
                p.tt("dve", g2_, ex, g1, ALU.mult, R=G, W=G)
                p.ts("dve", gt, mk1, g1, None, ALU.mult, R=G, W=G)
                p.stt(gates[:, ti, :], mk2, g2_, gt, ALU.mult, ALU.add, R=G, W=[f"gates.{ti}"])
        for e in range(E if dbg != 1 else 0):
            for (j0, nf) in UNITS:
                first_acc = (e == 0 and j0 == 0)
                wb = nunit % 2
                nunit += 1
                wtok = f"w{wb}"
                p.dma("pool", wgs[wb][:, :, 0:nf * 128],
                      wg[e].rearrange("(kc p) f -> p kc f", p=128)[:, :, j0 * 128:(j0 + nf) * 128], W=[wtok + "g"])
                p.dma("pool", wus[wb][:, :, 0:nf * 128],
                      wu[e].rearrange("(kc p) f -> p kc f", p=128)[:, :, j0 * 128:(j0 + nf) * 128], W=[wtok + "u"])
                p.dma("pool", wds[wb][:, 0:nf, :],
                      wd[e].rearrange("(j p) d -> p j d", p=128)[:, j0:j0 + nf, :], W=[wtok + "d"])
                abase = c.setdefault("nab", 0)
                c["nab"] = abase + NG

                def gu_(g, e=e, nf=nf, wb=wb, wtok=wtok, abase=abase):
                    ab = (abase + g) % 2
                    for j in range(nf):
                        k = c.setdefault("ngu", 0)
                        c["ngu"] = k + 1
                        gb = banks[2 + (k % 2)]
                        ub = banks[4 + (k % 2)]
                        gtok, utok = f"bank{2 + k % 2}", f"bank{4 + k % 2}"
                        for kc in range(8):
                            p.mm(gb[:, :], wgs[wb][:, kc, j * 128:(j + 1) * 128], hT[:, kc, g * 512:(g + 1) * 512],
                                 kc == 0, kc == 7,
                                 R=[wtok + "g"] + [f"hT.{g * 4 + t}.{kc}" for t in range(4)], W=[gtok])
                        for kc in range(8):
                            p.mm(ub[:, :], wus[wb][:, kc, j * 128:(j + 1) * 128], hT[:, kc, g * 512:(g + 1) * 512],
                                 kc == 0, kc == 7,
                                 R=[wtok + "u"] + [f"hT.{g * 4 + t}.{kc}" for t in range(4)], W=[utok])
                        sb_ = k % 2
                        p.act(sl[sb_][:, :], gb[:, :], AF.Silu, R=[gtok], W=[f"sl{sb_}"])
                        p.tt("dve", aT[ab][:, j, :], sl[sb_][:, :], ub[:, :], ALU.mult,
                             R=[f"sl{sb_}", utok], W=[f"aT{ab}.{j}"])

                def dn_(g, e=e, nf=nf, wb=wb, wtok=wtok, abase=abase, first_acc=first_acc):
                    ab = (abase + g) % 2
                    for t in range(4):
                        ti = g * 4 + t
                        for half in range(2):
                            k = c.setdefault("ndn", 0)
                            c["ndn"] = k + 1
                            db = banks[6 + (k % 2)]
                            dtok = f"bank{6 + k % 2}"
                            for j in range(nf):
                                p.mm(db[:, :], aT[ab][:, j, t * 128:(t + 1) * 128],
                                     wds[wb][:, j, half * 512:(half + 1) * 512], j == 0, j == nf - 1,
                                     R=[f"aT{ab}.{j}", wtok + "d"], W=[dtok])
                            dst = acc[:, ti, half * 512:(half + 1) * 512]
                            atok = f"acc.{ti}.{half}"
                            if route:
                                gsl = gates[:, ti, e:e + 1]
                                if first_acc:
                                    p.ts("dve", dst, db[:, :], gsl, None, ALU.mult,
                                         R=[dtok, f"gates.{ti}"], W=[atok])
                                else:
                                    p.stt(dst, db[:, :], gsl, dst, ALU.mult, ALU.add,
                                          R=[dtok, f"gates.{ti}", atok], W=[atok])
                            else:
                                if first_acc:
                                    p.copy("dve", dst, db[:, :], R=[dtok], W=[atok])
                                else:
                                    p.tt("dve", dst, db[:, :], dst, ALU.add, R=[dtok, atok], W=[atok])

                gu_(0)
                for g in range(NG):
                    if g + 1 < NG:
                        gu_(g + 1)
                    dn_(g)
        for ti in range(NT):
            b = nld % 2
            nld += 1
            p.dma("sp", xt[b][:, :], x_in[tok0 + ti * 128: tok0 + (ti + 1) * 128, :], W=[f"xt{b}"])
            at = [f"acc.{ti}.0", f"acc.{ti}.1"]
            p.tt("dve", acc[:, ti, :], acc[:, ti, :], g2b[:, :], ALU.mult, R=at + ["g2b"], W=at)
            p.tt("pool", acc[:, ti, :], acc[:, ti, :], xt[b][:, :], ALU.add, R=at + [f"xt{b}"], W=at)
            if final:
                k = c["nt"]
                c["nt"] = k + 1
                sb_ = k % 2
                s_ = ss[sb_]
                st = f"ss{sb_}"
                p.act(xn[sb_][:, :], acc[:, ti, :], AF.Square, accum_out=s_[:, 0:1], R=at, W=[f"xn{sb_}", st])
                p.ts("dve", s_[:, 1:2], s_[:, 0:1], 1.0 / D, EPS, ALU.mult, ALU.add, R=[st], W=[st])
                p.act(s_[:, 2:3], s_[:, 1:2], AF.Sqrt, R=[st], W=[st])
                p.recip(s_[:, 3:4], s_[:, 2:3], R=[st], W=[st])
                p.stt(acc[:, ti, :], acc[:, ti, :], s_[:, 3:4], fgb[:, :], ALU.mult, ALU.mult,
                      R=at + [st, "fgb"], W=at)
            p.dma("sp", y_out[tok0 + ti * 128: tok0 + (ti + 1) * 128, :], acc[:, ti, :], R=at, is_out=True)


TWO_PI = float(2.0 * np.pi)
C1 = 6.28125
C2 = float(2.0 * np.pi - 6.28125)
PI32 = float(np.float32(np.pi))


def build_mod(T, L=4, **kw):
    p = Prog()
    emit_mod(p, T, L, **kw)
    return p.finish()


def emit_mod(p, T, L=4, **kw):
    c_in = p.din("c", [128, 8])
    adaw = p.din("ada_w", [L, D, 6 * D])
    adab = p.din("ada_b", [L, 128, 6 * D])
    ngb = p.din("ngb", [L, 2, 128, D])
    ones_in = p.din("ones", [128, 128])
    posb = p.din("posb", [128, T], I32)
    invf = p.din("invf", [128, 2])
    modv = p.dout("modv", [L, 6, 128, D])
    cos_o = p.dout("cos2", [128, T])
    sin_o = p.dout("sin2s", [128, T])

    ones = p.sb("ones", [128, 128])
    cond = p.sb("cond", [128, 8])
    crep = p.sb("crep", [128, 8, 128])
    wt = [p.sb(f"wt{i}", [128, 8, 512]) for i in range(2)]
    bb = p.sb("bb", [128, 6 * D])
    modb = p.sb("modb", [128, 6 * D])
    ng = p.sb("ng", [128, 2, D])
    ao = [p.sb(f"ao{i}", [128, D]) for i in range(2)]
    banks = [p.ps(f"bank{i}") for i in range(2)]

    p.dma("sp", ones[:, :], ones_in, W=["ones"])
    p.dma("sp", cond[:, :], c_in, W=["cond"])
    p.act(cond[:, :], cond[:, :], AF.Silu, R=["cond"], W=["cond"])
    for kc in range(8):
        p.ts("dve", crep[:, kc, :], ones[:, :], cond[:, kc:kc + 1], None, ALU.mult, R=["ones", "cond"], W=["crep"])
    nw = 0
    for l in range(L):
        p.dma("act", bb[:, :], adab[l], W=["bb"])
        p.dma("act", ng[:, :, :], ngb[l].rearrange("t p d -> p t d"), W=["ng"])
        for n in range(12):
            b = nw % 2
            nw += 1
            p.dma("sp", wt[b][:, :, :], adaw[l].rearrange("(kc p) n -> p kc n", p=128)[:, :, n * 512:(n + 1) * 512],
                  W=[f"wt{b}"])
            for kc in range(8):
                p.mm(banks[b][:, :], crep[:, kc, :], wt[b][:, kc, :], kc == 0, kc == 7,
                     R=["crep", f"wt{b}"], W=[f"bank{b}"])
            p.tt("dve", modb[:, n * 512:(n + 1) * 512], banks[b][:, :], bb[:, n * 512:(n + 1) * 512], ALU.add,
                 R=[f"bank{b}", "bb"], W=[f"modb{n // 2}"])
        for s in range(2):
            p.stt(ao[s][:, :], modb[:, (3 * s + 1) * D:(3 * s + 2) * D], 1.0, ng[:, s, :], ALU.add, ALU.mult,
                  R=[f"modb{3 * s + 1}", "ng"], W=[f"ao{s}"])
            p.dma("act", modv[l, 3 * s + 0], ao[s][:, :], R=[f"ao{s}"], is_out=True)
            p.dma("act", modv[l, 3 * s + 1], modb[:, (3 * s) * D:(3 * s + 1) * D], R=[f"modb{3 * s}"], is_out=True)
            p.dma("act", modv[l, 3 * s + 2], modb[:, (3 * s + 2) * D:(3 * s + 3) * D], R=[f"modb{3 * s + 2}"],
                  is_out=True)

    CH = min(T, 2048)
    fq = p.sb("fq", [128, 2])
    pi_ = p.sb("pi", [128, CH], I32)
    ang = p.sb("ang", [128, CH])
    kr = p.sb("kr", [128, CH])
    ki = p.sb("ki", [128, CH], I32)
    r = p.sb("r", [128, CH])
    m = p.sb("m", [128, CH])
    so = p.sb("so", [128, CH])
    p.dma("sp", fq[:, :], invf, W=["fq"])
    for ch in range(T // CH):
        sl_ = slice(ch * CH, (ch + 1) * CH)
        p.dma("sp", pi_[:, :], posb[:, sl_], W=["pi"])
        p.copy("dve", ang[:, :], pi_[:, :], R=["pi"], W=["ang"])
        p.ts("dve", ang[:, :], ang[:, :], fq[:, 0:1], None, ALU.mult, R=["ang", "fq"], W=["ang"])
        p.ts("dve", kr[:, :], ang[:, :], 1.0 / TWO_PI, None, ALU.mult, R=["ang"], W=["kr"])
        p.copy("dve", ki[:, :], kr[:, :], R=["kr"], W=["ki"])
        p.copy("dve", kr[:, :], ki[:, :], R=["ki"], W=["kr"])
        p.stt(r[:, :], kr[:, :], -C1, ang[:, :], ALU.mult, ALU.add, R=["kr", "ang"], W=["r"])
        p.stt(r[:, :], kr[:, :], -C2, r[:, :], ALU.mult, ALU.add, R=["kr", "r"], W=["r"])
        p.ts("dve", m[:, :], r[:, :], PI32, None, ALU.is_gt, R=["r"], W=["m"])
        p.stt(r[:, :], m[:, :], -TWO_PI, r[:, :], ALU.mult, ALU.add, R=["m", "r"], W=["r"])
        p.ts("dve", m[:, :], r[:, :], -PI32, None, ALU.is_lt, R=["r"], W=["m"])
        p.stt(r[:, :], m[:, :], TWO_PI, r[:, :], ALU.mult, ALU.add, R=["m", "r"], W=["r"])
        p.act(so[:, :], r[:, :], AF.Sin, R=["r"], W=["so"])
        p.ts("dve", so[:, :], so[:, :], fq[:, 1:2], None, ALU.mult, R=["so", "fq"], W=["so"])
        p.dma("sp", sin_o[:, sl_], so[:, :], R=["so"], is_out=True)
        p.ts("dve", r[:, :], r[:, :], float(np.pi / 2), None, ALU.add, R=["r"], W=["r"])
        p.ts("dve", m[:, :], r[:, :], PI32, None, ALU.is_gt, R=["r"], W=["m"])
        p.stt(r[:, :], m[:, :], -TWO_PI, r[:, :], ALU.mult, ALU.add, R=["m", "r"], W=["r"])
        p.act(so[:, :], r[:, :], AF.Sin, R=["r"], W=["so"])
        p.dma("sp", cos_o[:, sl_], so[:, :], R=["so"], is_out=True)


def rope_consts():
    inv = (10000.0 ** (-np.arange(0, 32, 2, dtype=np.float32) / 32)).astype(np.float32)
    t = np.zeros((128, 2), np.float32)
    for r_ in range(128):
        j = r_ % 32
        t[r_, 0] = inv[j % 16]
        t[r_, 1] = -1.0 if j < 16 else 1.0
    return t


def build_mla_pre(T, **kw):
    p = Prog()
    emit_mla_pre(p, T, **kw)
    return p.finish()


def emit_mla_pre(p, T, **kw):
    x_in = p.din("x", [T, D])
    a_in = p.din("a1", [128, 8])
    sh_in = p.din("sh1", [128, 8])
    win_d = p.din("w_in", [D, 672])
    qn_in = p.din("qn", [128, 3])
    kvn_in = p.din("kvn", [128, 2])
    wuq_d = p.din("w_uq", [384, 1536])
    wukv_d = p.din("w_ukv", [256, 2048])
    cos_d = p.din("cos2", [128, T])
    sin_d = p.din("sin2s", [128, T])
    ident_in = p.din("ident", [128, 128])
    QN = p.dout("QN", [8, 128, T], BF16)
    QR = p.dout("QR", [4, 128, T], BF16)
    KN = p.dout("KN", [8, 128, T], BF16)
    KR = p.dout("KR", [32, T], BF16)
    V = p.dout("V", [T, D], BF16)

    NG = T // 512
    ident = p.sb("ident", [128, 128])
    identb = p.sb("identb", [128, 128], BF16)
    a_sb = p.sb("a_sb", [128, 8])
    sh_sb = p.sb("sh_sb", [128, 8])
    gn = p.sb("gn", [128, 5])
    win = p.sb("win", [128, 8, 672], BF16)
    winS = p.sb("winS", [128, 8, 32], BF16)
    wqn = p.sb("wqn", [128, 3, 1024], BF16)
    wqr = p.sb("wqr", [128, 3, 512], BF16)
    wqs = p.sb("wqs", [128, 3, 512], BF16)
    wkn = p.sb("wkn", [128, 2, 1024], BF16)
    wv = p.sb("wv", [128, 2, 1024], BF16)
    hT = [p.sb(f"hT{i}", [128, 8, 512], BF16) for i in range(2)]
    cT = [p.sb(f"cT{i}", [128, 5, 512], BF16) for i in range(2)]
    xt = [p.sb(f"xt{i}", [128, D]) for i in range(2)]
    xn = [p.sb(f"xn{i}", [128, D]) for i in range(2)]
    ss = [p.sb(f"ss{i}", [128, 4]) for i in range(2)]
    s2 = [p.sb(f"s2{i}", [128, 8]) for i in range(2)]
    junk = p.sb("junk", [128, 384])
    cqk = [p.sb(f"cqk{i}", [128, 640], BF16) for i in range(2)]
    cs = [p.sb(f"cs{i}", [128, 512]) for i in range(2)]
    sn = [p.sb(f"sn{i}", [128, 512]) for i in range(2)]
    t1 = [p.sb(f"t1{i}", [128, 512]) for i in range(2)]
    t2 = [p.sb(f"t2{i}", [128, 512]) for i in range(2)]
    stg = [p.sb(f"stg{i}", [128, 512], BF16) for i in range(4)]
    banks = [p.ps(f"bank{i}") for i in range(4)] + [None] + [p.ps(f"bank{i}") for i in range(5, 8)]
    trb = p.ps("trb", [128, 1024], BF16)

    p.dma("sp", ident[:, :], ident_in, W=["ident"])
    p.copy("dve", identb[:, :], ident[:, :], R=["ident"], W=["identb"])
    p.dma("sp", a_sb[:, :], a_in, W=["modv"])
    p.dma("sp", sh_sb[:, :], sh_in, W=["modv"])
    p.dma("sp", gn[:, 0:3], qn_in, W=["gn"])
    p.dma("sp", gn[:, 3:5], kvn_in, W=["gn"])
    p.dma("pool", win[:, :, :], win_d.rearrange("(kc p) f -> p kc f", p=128), W=["win"])
    p.dma("pool", winS[:, :, 0:16], win_d.rearrange("(kc p) f -> p kc f", p=128)[:, :, 656:672], W=["winS"])
    p.dma("pool", winS[:, :, 16:32], win_d.rearrange("(kc p) f -> p kc f", p=128)[:, :, 640:656], W=["winS"])
    uq4 = wuq_d.rearrange("(kc p) (h d) -> p kc h d", p=128, d=96)
    ukv4 = wukv_d.rearrange("(kc p) (h d) -> p kc h d", p=128, d=128)
    for kc in range(3):
        p.dma("pool", wqn[:, kc, :].rearrange("p (h d) -> p h d", d=64), uq4[:, kc, :, 0:64], W=["wqn"])
        p.dma("pool", wqr[:, kc, :].rearrange("p (h d) -> p h d", d=32), uq4[:, kc, :, 64:96], W=["wqr"])
        p.dma("pool", wqs[:, kc, :].rearrange("p (h d) -> p h d", d=32)[:, :, 0:16], uq4[:, kc, :, 80:96], W=["wqs"])
        p.dma("pool", wqs[:, kc, :].rearrange("p (h d) -> p h d", d=32)[:, :, 16:32], uq4[:, kc, :, 64:80], W=["wqs"])
    for kc in range(2):
        p.dma("pool", wkn[:, kc, :].rearrange("p (h d) -> p h d", d=64), ukv4[:, kc, :, 0:64], W=["wkn"])
        p.dma("pool", wv[:, kc, :].rearrange("p (h d) -> p h d", d=64), ukv4[:, kc, :, 64:128], W=["wv"])

    c = {"nt": 0, "xn": xn, "ss": ss, "tp": [banks[0], banks[1]], "tp_tok": ["bank0", "bank1"],
         "ident": ident, "a": a_sb, "sh": sh_sb}
    cnt = {"ob": 0, "stg": 0, "ld": 0}

    def obank():
        k = cnt["ob"]
        cnt["ob"] = k + 1
        i = 5 + k % 3
        return banks[i], f"bank{i}"

    def stage():
        k = cnt["stg"]
        cnt["stg"] = k + 1
        return stg[k % 4], f"stg{k % 4}", k

    for g in range(NG):
        hb = g % 2
        h_ = hT[hb]
        c_ = cT[hb]
        tok0 = g * 512
        p.dma("act", cs[hb][:, :], cos_d[:, tok0:tok0 + 512], W=[f"cs{hb}"])
        p.dma("act", sn[hb][:, :], sin_d[:, tok0:tok0 + 512], W=[f"sn{hb}"])
        for t in range(4):
            b = cnt["ld"] % 2
            cnt["ld"] += 1
            p.dma("sp", xt[b][:, :], x_in[tok0 + t * 128: tok0 + (t + 1) * 128, :], W=[f"xt{b}"])
            emit_norm_tile(p, c, xt[b], f"xt{b}", t, h_, f"hT{hb}")
            hR = [f"hT{hb}.{t}.{kc}" for kc in range(8)]
            for kc in range(8):
                p.mm(banks[2][:, 0:384], h_[:, kc, t * 128:(t + 1) * 128], win[:, kc, 0:384], kc == 0, kc == 7,
                     R=hR + ["win"], W=["bank2"])
            for kc in range(8):
                p.mm(banks[3][:, 0:256], h_[:, kc, t * 128:(t + 1) * 128], win[:, kc, 384:640], kc == 0, kc == 7,
                     R=hR + ["win"], W=["bank3"])
            sb_ = b
            s_ = s2[sb_]
            st = f"s2{sb_}"
            p.act(junk[:, 0:384], banks[2][:, 0:384], AF.Square, scale=float(384 ** -0.5), accum_out=s_[:, 0:1],
                  R=["bank2"], W=["junk", st])
            p.act(junk[:, 0:256], banks[3][:, 0:256], AF.Square, scale=float(256 ** -0.5), accum_out=s_[:, 1:2],
                  R=["bank3"], W=["junk", st])
            p.ts("dve", s_[:, 2:4], s_[:, 0:2], EPS, None, ALU.add, R=[st], W=[st])
            p.act(s_[:, 4:6], s_[:, 2:4], AF.Sqrt, R=[st], W=[st])
            p.recip(s_[:, 6:8], s_[:, 4:6], R=[st], W=[st])
            cq = cqk[sb_]
            ctok = f"cqk{sb_}"
            p.ts("dve", cq[:, 0:384], banks[2][:, 0:384], s_[:, 6:7], None, ALU.mult, R=["bank2", st], W=[ctok])
            p.ts("dve", cq[:, 384:640], banks[3][:, 0:256], s_[:, 7:8], None, ALU.mult, R=["bank3", st], W=[ctok])
            for q in range(5):
                p.tr(trb[:, q * 128:(q + 1) * 128], cq[:, q * 128:(q + 1) * 128], identb[:, :],
                     R=[ctok, "identb"], W=["trb"])
            for q in range(5):
                p.act(c_[:, q, t * 128:(t + 1) * 128], trb[:, q * 128:(q + 1) * 128], AF.Copy,
                      scale=gn[:, q:q + 1], R=["trb", "gn"], W=[f"cT{hb}.{t}"])
        cR = [f"cT{hb}.{t}" for t in range(4)]
        hRall = [f"hT{hb}.{t}.{kc}" for t in range(4) for kc in range(8)]
        for which, wsb, wtok, nk, koff, dst in (("qn", wqn, "wqn", 3, 0, QN), ("kn", wkn, "wkn", 2, 3, KN)):
            for ch in range(8):
                bk, btok = obank()
                for kc in range(nk):
                    p.mm(bk[:, :], wsb[:, kc, ch * 128:(ch + 1) * 128], c_[:, koff + kc, :], kc == 0, kc == nk - 1,
                         R=cR + [wtok], W=[btok])
                sg, stok, k = stage()
                if k % 2 == 0:
                    p.act(sg[:, :], bk[:, :], AF.Copy, R=[btok], W=[stok])
                else:
                    p.copy("dve", sg[:, :], bk[:, :], R=[btok], W=[stok])
                p.dma("sp", dst[ch, :, tok0:tok0 + 512], sg[:, :], R=[stok], is_out=True)
        for t in range(4):
            for half in range(2):
                bk, btok = obank()
                for kc in range(2):
                    p.mm(bk[:, :], c_[:, 3 + kc, t * 128:(t + 1) * 128], wv[:, kc, half * 512:(half + 1) * 512],
                         kc == 0, kc == 1, R=[f"cT{hb}.{t}", "wv"], W=[btok])
                sg, stok, k = stage()
                if k % 2 == 0:
                    p.act(sg[:, :], bk[:, :], AF.Copy, R=[btok], W=[stok])
                else:
                    p.copy("dve", sg[:, :], bk[:, :], R=[btok], W=[stok])
                p.dma("sp", V[tok0 + t * 128: tok0 + (t + 1) * 128, half * 512:(half + 1) * 512], sg[:, :],
                      R=[stok], is_out=True)
        jobs = [(wqr[:, :, ch * 128:(ch + 1) * 128], wqs[:, :, ch * 128:(ch + 1) * 128], 3, 0, c_, cR, 128,
                 QR[ch, :, tok0:tok0 + 512], ["wqr", "wqs"]) for ch in range(4)]
        jobs.append((win[:, :, 640:672], winS[:, :, :], 8, 0, h_, hRall, 32, KR[:, tok0:tok0 + 512], ["win", "winS"]))
        for (wa, ws_, nk, koff, src, sR, M, dst, wR) in jobs:
            b1, b1t = obank()
            for kc in range(nk):
                p.mm(b1[0:M, :], wa[:, kc, :], src[:, koff + kc, :], kc == 0, kc == nk - 1, R=sR + wR, W=[b1t])
            b2, b2t = obank()
            for kc in range(nk):
                p.mm(b2[0:M, :], ws_[:, kc, :], src[:, koff + kc, :], kc == 0, kc == nk - 1, R=sR + wR, W=[b2t])
            k = cnt.setdefault("rp", 0)
            cnt["rp"] = k + 1
            tb = k % 2
            p.tt("dve", t1[tb][0:M, :], b1[0:M, :], cs[hb][0:M, :], ALU.mult, R=[b1t, f"cs{hb}"], W=[f"t1{tb}"])
            p.tt("dve", t2[tb][0:M, :], b2[0:M, :], sn[hb][0:M, :], ALU.mult, R=[b2t, f"sn{hb}"], W=[f"t2{tb}"])
            sg, stok, _ = stage()
            p.tt("pool", sg[0:M, :], t1[tb][0:M, :], t2[tb][0:M, :], ALU.add, R=[f"t1{tb}", f"t2{tb}"], W=[stok])
            p.dma("sp", dst, sg[0:M, :], R=[stok], is_out=True)


def build_attn(S, NH, **kw):
    p = Prog()
    emit_attn(p, S, NH, **kw)
    return p.finish()


def emit_attn(p, S, NH, src=None, **kw):
    if src is None:
        QT = p.din("QT", [NH, 96, S], BF16)
        KT = p.din("KT", [NH, 96, S], BF16)
        Vd = p.din("V", [NH, 128, (S // 128) * 64], BF16)
    ones_in = p.din("ones", [128, 64], BF16)
    OT = p.dout("OT", [NH // 2, 128, S], BF16)
    NKT = S // 128
    NG = S // 512
    scale = float(96 ** -0.5)

    ones = p.sb("ones", [128, 64], BF16)
    q_sb = [p.sb(f"q{i}", [96, S], BF16) for i in range(2)]
    k_sb = [p.sb(f"k{i}", [96, S], BF16) for i in range(2)]
    v_sb = [p.sb(f"v{i}", [128, NKT * 64], BF16) for i in range(2)]
    NB = 4
    LA = 2
    pT = [p.sb(f"pT{i}", [128, 512], BF16) for i in range(NB)]
    rl = [p.sb(f"rl{i}", [64, 512]) for i in range(2)]
    og = [p.sb(f"og{i}", [64, 512], BF16) for i in range(2)]
    sbk = [p.ps(f"sb{i}") for i in range(NB)]
    obk = [p.ps(f"ob{i}") for i in range(2)]
    lbk = [p.ps(f"lb{i}") for i in range(2)]
    p.dma("sp", ones[:, :], ones_in, W=["ones"])

    def load_head(h):
        hb = h % 2
        if src is None:
            p.dma("sp", q_sb[hb][:, :], QT[h], W=[f"q{hb}"])
            p.dma("sp", k_sb[hb][:, :], KT[h], W=[f"k{hb}"])
            p.dma("sp", v_sb[hb][:, :], Vd[h], W=[f"v{hb}"])
        else:
            r2 = slice((h % 2) * 64, (h % 2) * 64 + 64)
            r4 = slice((h % 4) * 32, (h % 4) * 32 + 32)
            p.dma("sp", q_sb[hb][0:64, :], src["QN"][h // 2, r2, :], W=[f"q{hb}"])
            p.dma("sp", q_sb[hb][64:96, :], src["QR"][h // 4, r4, :], W=[f"q{hb}"])
            p.dma("sp", k_sb[hb][0:64, :], src["KN"][h // 2, r2, :], W=[f"k{hb}"])
            p.dma("sp", k_sb[hb][64:96, :], src["KR"], W=[f"k{hb}"])
            vsrc = src["V"].rearrange("(kt p) d -> p kt d", p=128)
            KQ = min(8, NKT)
            for kq in range(0, NKT, KQ):
                p.dma("act", v_sb[hb][:, kq * 64:(kq + KQ) * 64].rearrange("p (kt d) -> p kt d", d=64),
                      vsrc[:, kq:kq + KQ, h * 64:(h + 1) * 64], W=[f"v{hb}"])

    its = [(h, g, kt) for h in range(NH) for g in range(NG) for kt in range(4 * (g + 1))]
    N = len(its)
    load_head(0)
    for n in range(N + LA):
        if n < N:
            h, g, kt = its[n]
            hb = h % 2
            sb_ = n % NB
            p.mm(sbk[sb_][:, :], k_sb[hb][:, kt * 128:(kt + 1) * 128], q_sb[hb][:, g * 512:(g + 1) * 512],
                 True, True, R=[f"k{hb}", f"q{hb}"], W=[f"sb{sb_}"])
            p.act(pT[sb_][:, :], sbk[sb_][:, :], AF.Exp, scale=scale, R=[f"sb{sb_}"], W=[f"pT{sb_}"])
            i = kt - 4 * g
            if i >= 0:
                if i > 0:
                    p.memset("pool", pT[sb_][0:64, 0:128 * i], 0.0, W=[f"pT{sb_}"])
                p.memset("pool", pT[sb_][64:128, 0:128 * i + 64], 0.0, W=[f"pT{sb_}"])
        m = n - LA
        if m >= 0:
            h, g, kt = its[m]
            hb = h % 2
            gb = (h * NG + g) % 2
            nkt = 4 * (g + 1)
            sb_ = m % NB
            p.mm(obk[gb][0:64, :], v_sb[hb][:, kt * 64:(kt + 1) * 64], pT[sb_][:, :], kt == 0, kt == nkt - 1,
                 R=[f"v{hb}", f"pT{sb_}"], W=[f"ob{gb}"])
            p.mm(lbk[gb][0:64, :], ones[:, :], pT[sb_][:, :], kt == 0, kt == nkt - 1,
                 R=["ones", f"pT{sb_}"], W=[f"lb{gb}"])
            if g == 0 and kt == 0 and h + 1 < NH:
                load_head(h + 1)
            if kt == nkt - 1:
                p.recip(rl[gb][:, :], lbk[gb][0:64, :], R=[f"lb{gb}"], W=[f"rl{gb}"])
                p.tt("dve", og[gb][:, :], obk[gb][0:64, :], rl[gb][:, :], ALU.mult, R=[f"ob{gb}", f"rl{gb}"],
                     W=[f"og{gb}"])
                p.dma("act", OT[h // 2, (h % 2) * 64:(h % 2) * 64 + 64, g * 512:(g + 1) * 512], og[gb][:, :],
                      R=[f"og{gb}"], is_out=True)


def build_outproj(T, KC, **kw):
    p = Prog()
    emit_outproj(p, T, KC, **kw)
    return p.finish()


def emit_outproj(p, T, KC, **kw):
    x_in = p.din("x", [T, D])
    ot_in = p.din("OT", [KC, 128, T], BF16)
    w_in = p.din("w_out", [KC * 128, D])
    g1b_in = p.din("g1b", [128, D])
    y_out = p.dout("y", [T, D])
    w = p.sb("w", [128, KC, D], BF16)
    g1b = p.sb("g1b", [128, D])
    ot = [p.sb(f"ot{i}", [128, KC, 512], BF16) for i in range(2)]
    xt = [p.sb(f"xt{i}", [128, D]) for i in range(3)]
    tmp = [p.sb(f"tmp{i}", [128, 512]) for i in range(2)]
    banks = [p.ps(f"bank{i}") for i in range(4)]
    wv_ = w_in.rearrange("(kc p) d -> p kc d", p=128)
    for k0 in range(0, KC, 4):
        p.dma("pool", w[:, k0:k0 + 4, :], wv_[:, k0:k0 + 4, :], W=["w"])
    p.dma("sp", g1b[:, :], g1b_in, W=["g1b"])
    n = 0
    nx = 0
    for g in range(T // 512):
        gb = g % 2
        p.dma("act", ot[gb][:, :, :], ot_in[:, :, g * 512:(g + 1) * 512].rearrange("k p t -> p k t"), W=[f"ot{gb}"])
        for t in range(4):
            xb = nx % 3
            nx += 1
            r0 = g * 512 + t * 128
            p.dma("sp", xt[xb][:, :], x_in[r0:r0 + 128, :], W=[f"xt{xb}"])
            for half in range(2):
                bi = n % 4
                tb = n % 2
                n += 1
                for kc in range(KC):
                    p.mm(banks[bi][:, :], ot[gb][:, kc, t * 128:(t + 1) * 128], w[:, kc, half * 512:(half + 1) * 512],
                         kc == 0, kc == KC - 1, R=[f"ot{gb}", "w"], W=[f"bank{bi}"])
                p.tt("dve", tmp[tb][:, :], banks[bi][:, :], g1b[:, half * 512:(half + 1) * 512], ALU.mult,
                     R=[f"bank{bi}", "g1b"], W=[f"tmp{tb}"])
                p.tt("pool", xt[xb][:, half * 512:(half + 1) * 512], xt[xb][:, half * 512:(half + 1) * 512],
                     tmp[tb][:, :], ALU.add, R=[f"tmp{tb}", f"xt{xb}"], W=[f"xt{xb}"])
            p.dma("sp", y_out[r0:r0 + 128, :], xt[xb][:, :], R=[f"xt{xb}"], is_out=True)


SSM_IN = 5152


def build_ssm_pre(T, **kw):
    p = Prog()
    emit_ssm_pre(p, T, **kw)
    return p.finish()


def emit_ssm_pre(p, T, **kw):
    x_in = p.din("x", [T, D])
    a_in = p.din("a1", [128, 8])
    sh_in = p.din("sh1", [128, 8])
    w_d = p.din("w_in", [D, SSM_IN])
    ident_in = p.din("ident", [128, 128])
    fused = "PT24" in p.bind
    if fused:
        PT24 = p.bind["PT24"]
        ZT = p.bind["ZT"]
        DTt = p.bind["DTt"]
    else:
        PT = p.dout("PT", [40, 128, T], BF16)
        DT = p.dout("DT", [32, T])
    ident = p.sb("ident", [128, 128])
    a_sb = p.sb("a_sb", [128, 8])
    sh_sb = p.sb("sh_sb", [128, 8])
    w = p.sb("w", [128, 8, SSM_IN], BF16)
    hT = [p.sb(f"hT{i}", [128, 8, 512], BF16) for i in range(2)]
    xt = [p.sb(f"xt{i}", [128, D]) for i in range(2)]
    xn = [p.sb(f"xn{i}", [128, D]) for i in range(2)]
    ss = [p.sb(f"ss{i}", [128, 4]) for i in range(2)]
    stg = [p.sb(f"stg{i}", [128, 512], BF16) for i in range(4)]
    stf = p.sb("stf", [32, 512])
    stf2 = p.sb("stf2", [128, 32])
    banks = [p.ps(f"bank{i}") for i in range(6)]
    p.dma("sp", ident[:, :], ident_in, W=["ident"])
    p.dma("sp", a_sb[:, :], a_in, W=["modv"])
    p.dma("sp", sh_sb[:, :], sh_in, W=["modv"])
    wv_ = w_d.rearrange("(kc p) f -> p kc f", p=128)
    for kc in range(8):
        p.dma("pool", w[:, kc, :], wv_[:, kc, :], W=[f"w{kc}"])
    c = {"nt": 0, "xn": xn, "ss": ss, "tp": [banks[0], banks[1]], "tp_tok": ["bank0", "bank1"],
         "ident": ident, "a": a_sb, "sh": sh_sb}
    n = 0
    nld = 0
    for g in range(T // 512):
        hb = g % 2
        tok0 = g * 512
        for t in range(4):
            b = nld % 2
            nld += 1
            p.dma("sp", xt[b][:, :], x_in[tok0 + t * 128: tok0 + (t + 1) * 128, :], W=[f"xt{b}"])
            emit_norm_tile(p, c, xt[b], f"xt{b}", t, hT[hb], f"hT{hb}")
        hR = [f"hT{hb}.{t}.{kc}" for t in range(4) for kc in range(8)]
        if fused:
            for t in range(4):
                hRt = [f"hT{hb}.{t}.{kc}" for kc in range(8)]
                r0 = tok0 + t * 128
                for nq in range(5):
                    N = 512 if nq < 4 else 32
                    c0 = nq * 512 if nq < 4 else 5120
                    bi = 2 + n % 4
                    si = n % 4
                    n += 1
                    for kc in range(8):
                        p.mm(banks[bi][:, 0:N], hT[hb][:, kc, t * 128:(t + 1) * 128], w[:, kc, c0:c0 + N],
                             kc == 0, kc == 7, R=hRt + [f"w{kc}"], W=[f"bank{bi}"])
                    if nq < 4:
                        if n % 2 == 0:
                            p.act(stg[si][:, :], banks[bi][:, :], AF.Copy, R=[f"bank{bi}"], W=[f"stg{si}"])
                        else:
                            p.copy("dve", stg[si][:, :], banks[bi][:, :], R=[f"bank{bi}"], W=[f"stg{si}"])
                        p.dma("sp", ZT[r0:r0 + 128, c0:c0 + 512], stg[si][:, :], R=[f"stg{si}"], W=["ZT"])
                    else:
                        p.copy("dve", stf2[:, :], banks[bi][:, 0:32], R=[f"bank{bi}"], W=["stf2"])
                        p.dma("sp", DTt[r0:r0 + 128, :], stf2[:, :], R=["stf2"], W=["DTt"])
        for ch in (range(16, 40) if fused else range(41)):
            M = 128 if ch < 40 else 32
            bi = 2 + n % 4
            si = n % 4
            n += 1
            for kc in range(8):
                p.mm(banks[bi][0:M, :], w[:, kc, ch * 128: ch * 128 + M], hT[hb][:, kc, :], kc == 0, kc == 7,
                     R=hR + [f"w{kc}"], W=[f"bank{bi}"])
            if ch < 40:
                if n % 2 == 0:
                    p.act(stg[si][:, :], banks[bi][:, :], AF.Copy, R=[f"bank{bi}"], W=[f"stg{si}"])
                else:
                    p.copy("dve", stg[si][:, :], banks[bi][:, :], R=[f"bank{bi}"], W=[f"stg{si}"])
                if fused:
                    p.dma("sp", PT24[ch - 16, :, tok0:tok0 + 512], stg[si][:, :], R=[f"stg{si}"], W=["PT24"])
                else:
                    p.dma("sp", PT[ch, :, tok0:tok0 + 512], stg[si][:, :], R=[f"stg{si}"], is_out=True)
            else:
                p.copy("dve", stf[:, :], banks[bi][0:32, :], R=[f"bank{bi}"], W=["stf"])
                p.dma("sp", DT[:, tok0:tok0 + 512], stf[:, :], R=["stf"], is_out=True)


def build_ssm_core(S, **kw):
    p = Prog()
    emit_ssm_core(p, S, **kw)
    return p.finish()


def emit_ssm_core(p, S, **kw):
    NCH = S // 64
    fused = "PT24" in p.bind
    gp = kw.get("gp", 0)
    if fused:
        PT24 = p.bind["PT24"]
        zt = p.bind["ZT"][:, gp * 1024:(gp + 1) * 1024]
        dtr_d = p.bind["DTt"][:, gp * 16:(gp + 1) * 16].rearrange("(c l) h -> l c h", l=64)
        OTf = p.bind["OTf"]
    else:
        uT = p.din("uT", [12, 128, S + 3], BF16)
        zt = p.din("zt", [S, 1024], BF16)
        dtr_d = p.din("dtr", [64, NCH, 16])
    cw_d = p.din("cw", [128, 12, 4])
    cb_d = p.din("cb", [128, 12])
    dtb_d = p.din("dtb", [64, 16])
    alog_d = p.din("alog", [128, 16])
    dsk_d = p.din("dsk", [64, 16])
    ngb_d = p.din("ngb", [64, 1024])
    U_d = p.din("U", [64, 64])
    ones_d = p.din("ones64", [64, 128])
    I_d = p.din("I64", [64, 64])
    neg_d = p.din("NEGrep", [64, 512])
    idb_d = p.din("identb", [128, 128], BF16)
    if not fused:
        Y = p.dout("Y", [S, 1024], BF16)

    cw = p.sb("cw", [128, 12, 4])
    cb = p.sb("cb", [128, 12])
    dtb = p.sb("dtb", [64, 16])
    aB = p.sb("aB", [128, 16])
    dsk = p.sb("dsk", [64, 16])
    ngb = p.sb("ngb", [64, 1024])
    U = p.sb("U", [64, 64])
    ones64 = p.sb("ones64", [64, 128])
    I64 = p.sb("I64", [64, 64])
    NEG = p.sb("NEG", [64, 512])
    identb = p.sb("identb", [128, 128], BF16)
    dt_all = p.sb("dt_all", [64, NCH, 16])
    dtA_all = p.sb("dtA_all", [64, NCH, 16])
    ub = [p.sb(f"ub{i}", [128, 12, 515], BF16) for i in range(2)]
    xsT = [p.sb(f"xsT{i}", [128, 12, 512], BF16) for i in range(2)]
    acc = [p.sb(f"acc{i}", [128, 512]) for i in range(2)]
    xs = [p.sb(f"xs{i}", [64, 1024], BF16) for i in range(2)]
    Bt = [p.sb(f"Bt{i}", [64, 256], BF16) for i in range(2)]
    zb = [p.sb(f"zb{i}", [64, 1024], BF16) for i in range(2)]
    zs = p.sb("zs", [64, 1024])
    sm = p.sb("sm", [128, 96])
    R = p.sb("R", [64, 1024])
    segm = p.sb("segm", [64, 512])
    E = p.sb("E", [64, 512])
    Gs = p.sb("Gs", [64, 128])
    MT = p.sb("MT", [64, 512], BF16)
    xdt = p.sb("xdt", [64, 1024], BF16)
    xdte = p.sb("xdte", [64, 1024], BF16)
    yf = p.sb("yf", [64, 1024])
    t2 = p.sb("t2", [64, 1024])
    S32 = p.sb("S32", [128, 1024])
    Sb = p.sb("Sb", [128, 1024], BF16)
    junk = p.sb("junk", [64, 512])
    gst = p.sb("gst", [64, 8])
    Yst = [p.sb(f"Yst{i}", [64, 1024], BF16) for i in range(2)]
    YTs = [p.sb(f"YTs{i}", [128, 512], BF16) for i in range(2)]
    trx = p.ps("trx", [128, 1024], BF16)
    trB = p.ps("trB", [128, 1024], BF16)
    smp = p.ps("smp")
    gbk = p.ps("gbk")
    segb = p.ps("segb")
    ydb = p.ps("ydb")
    yob = p.ps("yob")
    stb = p.ps("stb")

    for (dst, src, tok) in ((cw[:, :, :], cw_d, "cw"), (cb[:, :], cb_d, "cb"), (dtb[:, :], dtb_d, "dtb"),
                            (aB[:, :], alog_d, "aB"), (dsk[:, :], dsk_d, "dsk"), (ngb[:, :], ngb_d, "ngb"),
                            (U[:, :], U_d, "U"), (ones64[:, :], ones_d, "ones64"), (I64[:, :], I_d, "I64"),
                            (NEG[:, :], neg_d, "NEG"), (identb[:, :], idb_d, "identb")):
        p.dma("sp", dst, src, W=[tok])
    for c0 in range(0, NCH, 8):
        p.dma("sp", dt_all[:, c0:c0 + 8, :], dtr_d[:, c0:c0 + 8, :], W=["dt_all"])
    p.act(aB[:, :], aB[:, :], AF.Exp, R=["aB"], W=["aB"])
    p.ts("dve", aB[:, :], aB[:, :], -1.0, None, ALU.mult, R=["aB"], W=["aB"])
    p.tt("dve", dt_all[:, :, :], dt_all[:, :, :], dtb[:, :].unsqueeze(1).broadcast_to([64, NCH, 16]), ALU.add,
         R=["dt_all", "dtb"], W=["dt_all"])
    p.act(dt_all[:, :, :], dt_all[:, :, :], AF.Exp, R=["dt_all"], W=["dt_all"])
    p.act(dt_all[:, :, :], dt_all[:, :, :], AF.Ln, bias=1.0, R=["dt_all"], W=["dt_all"])
    p.tt("dve", dtA_all[:, :, :], dt_all[:, :, :], aB[0:64, :].unsqueeze(1).broadcast_to([64, NCH, 16]), ALU.mult,
         R=["dt_all", "aB"], W=["dtA_all"])
    p.memset("dve", S32[:, :], 0.0, W=["S32"])
    p.memset("pool", Sb[:, :], 0.0, W=["Sb"])

    def b3(ap, n):
        return ap.unsqueeze(2).broadcast_to([64, n, 64])

    nacc = 0
    for blk in range(S // 512):
        bb = blk % 2
        tok0 = blk * 512
        if not fused:
            p.dma("sp", ub[bb][:, :, :], uT[:, :, tok0:tok0 + 515].rearrange("j p t -> p j t"), W=[f"ub{bb}"])
        else:
            lo = 3 if blk == 0 else 0
            if blk == 0:
                p.memset("pool", ub[bb][:, :, 0:3], 0.0, W=[f"ub{bb}"])
            for (d0, nchk, s0) in ((0, 8, gp * 8), (8, 2, 16 + 2 * gp), (10, 2, 20 + 2 * gp)):
                p.dma("sp", ub[bb][:, d0:d0 + nchk, lo:515],
                      PT24[s0:s0 + nchk, :, tok0 - 3 + lo:tok0 + 512].rearrange("j p t -> p j t"), W=[f"ub{bb}"])
        for j in range(12):
            ab = nacc % 2
            nacc += 1
            a_ = acc[ab]
            at = f"acc{ab}"
            p.ts("dve", a_[:, :], ub[bb][:, j, 0:512], cw[:, j, 0:1], None, ALU.mult, R=[f"ub{bb}", "cw"], W=[at])
            for k in range(1, 4):
                p.stt(a_[:, :], ub[bb][:, j, k:k + 512], cw[:, j, k:k + 1], a_[:, :], ALU.mult, ALU.add,
                      R=[f"ub{bb}", "cw", at], W=[at])
            p.act(xsT[bb][:, j, :], a_[:, :], AF.Silu, bias=cb[:, j:j + 1], R=[at, "cb"], W=[f"xsT{bb}.{j}"])
        for cc in range(8):
            c = blk * 8 + cc
            o = cc * 64
            cb2 = c % 2
            xR = [f"xsT{bb}.{j}" for j in range(8)]
            p.dma("act", zb[cb2][:, :], zt[c * 64:(c + 1) * 64, :], W=[f"zb{cb2}"])
            for j in range(8):
                p.tr(trx[0:64, j * 128:(j + 1) * 128], xsT[bb][:, j, o:o + 64], identb[:, :],
                     R=[f"xsT{bb}.{j}", "identb"], W=["trx"])
            for j in range(2):
                p.tr(trB[0:64, j * 128:(j + 1) * 128], xsT[bb][:, 8 + j, o:o + 64], identb[:, :],
                     R=[f"xsT{bb}.{8 + j}", "identb"], W=["trB"])
            p.act(xs[cb2][:, :], trx[0:64, :], AF.Copy, R=["trx"], W=[f"xs{cb2}"])
            p.copy("dve", Bt[cb2][:, :], trB[0:64, 0:256], R=["trB"], W=[f"Bt{cb2}"])
            dtA_c = dtA_all[:, c, :]
            dt_c = dt_all[:, c, :]
            p.mm(smp[0:64, 0:16], U[:, :], dtA_c, True, True, R=["U", "dtA_all"], W=["smp"])
            p.mm(smp[:, 16:32], ones64[:, :], dtA_c, True, True, R=["ones64", "dtA_all"], W=["smp"])
            acs = sm[0:64, 0:16]
            nacs = sm[0:64, 16:32]
            ea = sm[0:64, 32:48]
            dte = sm[0:64, 48:64]
            cdB = sm[:, 64:80]
            p.copy("dve", acs, smp[0:64, 0:16], R=["smp"], W=["sm.acs"])
            p.ts("dve", nacs, acs, -1.0, None, ALU.mult, R=["sm.acs"], W=["sm.nacs"])
            p.act(ea, acs, AF.Exp, R=["sm.acs"], W=["sm.ea"])
            p.tt("dve", dte, smp[0:64, 16:32], acs, ALU.subtract, R=["smp", "sm.acs"], W=["sm.dte"])
            p.act(dte, dte, AF.Exp, R=["sm.dte"], W=["sm.dte"])
            p.act(cdB, smp[:, 16:32], AF.Exp, R=["smp"], W=["sm.cd"])
            p.tt("dve", R[:, :].rearrange("p (h l) -> p h l", l=64), b3(dtA_c, 16),
                 U[:, :].unsqueeze(1).broadcast_to([64, 16, 64]), ALU.mult, R=["dtA_all", "U"], W=["R"])
            for lg in range(2):
                p.mm(gbk[0:64, lg * 64:(lg + 1) * 64], xsT[bb][:, 8 + lg, o:o + 64], xsT[bb][:, 10 + lg, o:o + 64],
                     True, True, R=[f"xsT{bb}.{8 + lg}", f"xsT{bb}.{10 + lg}"], W=["gbk"])
            p.copy("dve", Gs[:, :], gbk[0:64, 0:128], R=["gbk"], W=["Gs"])
            p.tt("pool", xdt[:, :].rearrange("p (h d) -> p h d", d=64), xs[cb2][:, :].rearrange("p (h d) -> p h d", d=64),
                 b3(dt_c, 16), ALU.mult, R=[f"xs{cb2}", "dt_all"], W=["xdt"])
            p.tt("pool", xdte[:, :].rearrange("p (h d) -> p h d", d=64), xdt[:, :].rearrange("p (h d) -> p h d", d=64),
                 b3(dte, 16), ALU.mult, R=["xdt", "sm.dte"], W=["xdte"])
            p.tt("pool", t2[:, :].rearrange("p (h d) -> p h d", d=64), xs[cb2][:, :].rearrange("p (h d) -> p h d", d=64),
                 b3(dsk[:, :], 16), ALU.mult, R=[f"xs{cb2}", "dsk"], W=["t2"])
            for hf in range(2):
                hs = slice(hf * 8, hf * 8 + 8)
                cs_ = slice(hf * 512, hf * 512 + 512)
                p.mm(segb[0:64, :], ones64[:, 0:64], R[:, cs_], True, False, R=["ones64", "R"], W=["segb"])
                p.mm(segb[0:64, :], I64[:, :], NEG[:, :], False, True, R=["I64", "NEG"], W=["segb"])
                p.tt("dve", segm[:, :].rearrange("p (h l) -> p h l", l=64),
                     segb[0:64, :].rearrange("p (h l) -> p h l", l=64), b3(sm[0:64, 16 + hf * 8:16 + hf * 8 + 8], 8),
                     ALU.add, R=["segb", "sm.nacs"], W=["segm"])
                p.act(E[:, :], segm[:, :], AF.Exp, R=["segm"], W=["E"])
                p.tt("dve", MT[:, :].rearrange("p (h l) -> p h l", l=64), E[:, :].rearrange("p (h l) -> p h l", l=64),
                     Gs[:, hf * 64:(hf + 1) * 64].unsqueeze(1).broadcast_to([64, 8, 64]), ALU.mult,
                     R=["E", "Gs"], W=["MT"])
                for hh in range(8):
                    h = hf * 8 + hh
                    p.mm(ydb[0:64, hh * 64:(hh + 1) * 64], MT[:, hh * 64:(hh + 1) * 64], xdt[:, h * 64:(h + 1) * 64],
                         True, True, R=["MT", "xdt"], W=["ydb"])
                for hh in range(8):
                    h = hf * 8 + hh
                    p.mm(yob[0:64, hh * 64:(hh + 1) * 64], xsT[bb][:, 10 + hf, o:o + 64], Sb[:, h * 64:(h + 1) * 64],
                         True, True, R=[f"xsT{bb}.{10 + hf}", f"Sb{hf}"], W=["yob"])
                for hh in range(8):
                    h = hf * 8 + hh
                    p.mm(stb[:, hh * 64:(hh + 1) * 64], Bt[cb2][:, hf * 128:(hf + 1) * 128], xdte[:, h * 64:(h + 1) * 64],
                         True, True, R=[f"Bt{cb2}", "xdte"], W=["stb"])
                yh = yf[:, cs_]
                p.tt("dve", yh.rearrange("p (h d) -> p h d", d=64), yob[0:64, :].rearrange("p (h d) -> p h d", d=64),
                     b3(sm[0:64, 32 + hf * 8:32 + hf * 8 + 8], 8), ALU.mult, R=["yob", "sm.ea"], W=[f"yf{hf}"])
                p.tt("dve", yh, yh, ydb[0:64, :], ALU.add, R=["ydb", f"yf{hf}"], W=[f"yf{hf}"])
                p.tt("pool", yh, yh, t2[:, cs_], ALU.add, R=["t2", f"yf{hf}"], W=[f"yf{hf}"])
                Sh = S32[:, cs_]
                p.tt("pool", Sh.rearrange("p (h d) -> p h d", d=64), Sh.rearrange("p (h d) -> p h d", d=64),
                     sm[:, 64 + hf * 8:64 + hf * 8 + 8].unsqueeze(2).broadcast_to([128, 8, 64]), ALU.mult,
                     R=[f"S32{hf}", "sm.cd"], W=[f"S32{hf}"])
                p.tt("dve", Sh, Sh, stb[:, :], ALU.add, R=["stb", f"S32{hf}"], W=[f"S32{hf}"])
                p.copy("pool", Sb[:, cs_], Sh, R=[f"S32{hf}"], W=[f"Sb{hf}"])
            p.act(zs[:, :], zb[cb2][:, :], AF.Silu, R=[f"zb{cb2}"], W=["zs"])
            p.tt("dve", yf[:, :], yf[:, :], zs[:, :], ALU.mult, R=["yf0", "yf1", "zs"], W=["yf0", "yf1"])
            for lg in range(2):
                p.act(junk[:, :], yf[:, lg * 512:(lg + 1) * 512], AF.Square, scale=float(512 ** -0.5),
                      accum_out=gst[:, lg:lg + 1], R=[f"yf{lg}"], W=["junk", "gst"])
            p.ts("dve", gst[:, 2:4], gst[:, 0:2], EPS, None, ALU.add, R=["gst"], W=["gst"])
            p.act(gst[:, 4:6], gst[:, 2:4], AF.Sqrt, R=["gst"], W=["gst"])
            p.recip(gst[:, 6:8], gst[:, 4:6], R=["gst"], W=["gst"])
            for lg in range(2):
                cs_ = slice(lg * 512, (lg + 1) * 512)
                p.stt(Yst[cb2][:, cs_], yf[:, cs_], gst[:, 6 + lg:7 + lg], ngb[:, cs_], ALU.mult, ALU.mult,
                      R=[f"yf{lg}", "gst", "ngb"], W=[f"Yst{cb2}"])
            if not fused:
                p.dma("sp", Y[c * 64:(c + 1) * 64, :], Yst[cb2][:, :], R=[f"Yst{cb2}"], is_out=True)
            else:
                for j in range(8):
                    p.tr(trx[:, j * 64:(j + 1) * 64], Yst[cb2][:, j * 128:(j + 1) * 128], identb[0:64, 0:64],
                         R=[f"Yst{cb2}", "identb"], W=["trx"])
                p.act(YTs[cb2][:, :], trx[:, 0:512], AF.Copy, R=["trx"], W=[f"YTs{cb2}"])
                p.dma("sp", OTf[gp * 8:(gp + 1) * 8, :, c * 64:(c + 1) * 64].rearrange("j p t -> p j t"),
                      YTs[cb2][:, :].rearrange("p (j t) -> p j t", t=64), R=[f"YTs{cb2}"], W=["OTf"])


def ssm_consts():
    k = np.arange(64)
    U = (k[:, None] <= k[None, :]).astype(np.float32)
    NEG = np.where(k[:, None] > k[None, :], -30000.0, 0.0).astype(np.float32)
    return {"U": U, "ones64": np.ones((64, 128), np.float32), "I64": np.eye(64, dtype=np.float32),
            "NEGrep": np.ascontiguousarray(np.tile(NEG, (1, 8)))}


B_, S_ = 4, 8192
L_ = 4


def build_fused(S=S_, L=L_):
    p = Prog()
    T = S
    ext = {}

    def E(name, shape, dt=F32):
        ext[name] = p.nc.dram_tensor(name, list(shape), dt, kind="ExternalInput").ap()
        return ext[name]

    x_in = E("x", [T, D])
    E("c", [128, 8])
    E("ada_w", [L, D, 6 * D])
    E("ada_b", [L, 128, 6 * D])
    E("ngb4", [L, 2, 128, D])
    E("ones32", [128, 128])
    E("posb", [128, T], I32)
    E("invf", [128, 2])
    E("ident", [128, 128])
    E("onesbf", [128, 64], BF16)
    E("identb", [128, 128], BF16)
    E("mla_w_in", [2, D, 672])
    E("qn", [2, 128, 3])
    E("kvn", [2, 128, 2])
    E("mla_w_uq", [2, 384, 1536])
    E("mla_w_ukv", [2, 256, 2048])
    E("mla_w_out", [2, 1024, D])
    E("ffn_wg", [2, 1, D, FF])
    E("ffn_wu", [2, 1, D, FF])
    E("ffn_wd", [2, 1, FF, D])
    E("ssm_w_in", [2, D, SSM_IN])
    E("cw", [2, 2, 128, 12, 4])
    E("cb", [2, 2, 128, 12])
    E("dtb", [2, 2, 64, 16])
    E("alog", [2, 2, 128, 16])
    E("dsk", [2, 2, 64, 16])
    E("sngb", [2, 2, 64, 1024])
    E("U", [64, 64])
    E("ones64", [64, 128])
    E("I64", [64, 64])
    E("NEGrep", [64, 512])
    E("ssm_w_out", [2, 2048, D])
    E("wr", [2, D, 8])
    E("moe_wg", [2, 8, D, FF])
    E("moe_wu", [2, 8, D, FF])
    E("moe_wd", [2, 8, FF, D])
    E("fgb", [128, D])
    out = p.nc.dram_tensor("out", [T, D], F32, kind="ExternalOutput").ap()

    sc = p.scratch
    modv = sc("modv", [L, 6, 128, D])
    cos2 = sc("cos2", [128, T])
    sin2s = sc("sin2s", [128, T])
    xa = sc("xa", [T, D])
    xb = sc("xb", [T, D])
    QN = sc("QN", [8, 128, T], BF16)
    QR = sc("QR", [4, 128, T], BF16)
    KN = sc("KN", [8, 128, T], BF16)
    KR = sc("KR", [32, T], BF16)
    V = sc("V", [T, D], BF16)
    OT8 = sc("OT8", [8, 128, T], BF16)
    OT16 = sc("OT16", [16, 128, T], BF16)
    PT24 = sc("PT24", [24, 128, T], BF16)
    ZT = sc("ZT", [T, 2048], BF16)
    DTt = sc("DTt", [T, 32])

    def fmv(l, i):
        return modv[l, i, 0, :].rearrange("(kc p) -> p kc", p=128)

    p.bind = {"c": ext["c"], "ada_w": ext["ada_w"], "ada_b": ext["ada_b"], "ngb": ext["ngb4"], "ones": ext["ones32"],
              "posb": ext["posb"], "invf": ext["invf"], "modv": modv, "cos2": cos2, "sin2s": sin2s}
    emit_mod(p, T, L)
    p.end_phase()

    xcur = x_in
    for l in range(L):
        j = l // 2
        if l % 2 == 0:
            p.bind = {"x": xcur, "a1": fmv(l, 0), "sh1": fmv(l, 1), "w_in": ext["mla_w_in"][j], "qn": ext["qn"][j],
                      "kvn": ext["kvn"][j], "w_uq": ext["mla_w_uq"][j], "w_ukv": ext["mla_w_ukv"][j], "cos2": cos2,
                      "sin2s": sin2s, "ident": ext["ident"], "QN": QN, "QR": QR, "KN": KN, "KR": KR, "V": V}
            emit_mla_pre(p, T)
            p.end_phase()
            p.bind = {"ones": ext["onesbf"], "OT": OT8}
            emit_attn(p, S, 16, src={"QN": QN, "QR": QR, "KN": KN, "KR": KR, "V": V})
            p.end_phase()
            KC, OT, w_out = 8, OT8, ext["mla_w_out"][j]
        else:
            p.bind = {"x": xcur, "a1": fmv(l, 0), "sh1": fmv(l, 1), "w_in": ext["ssm_w_in"][j], "ident": ext["ident"],
                      "PT24": PT24, "ZT": ZT, "DTt": DTt}
            emit_ssm_pre(p, T)
            p.end_phase()
            for gp in range(2):
                p.bind = {"PT24": PT24, "ZT": ZT, "DTt": DTt, "OTf": OT16, "cw": ext["cw"][j, gp], "cb": ext["cb"][j, gp],
                          "dtb": ext["dtb"][j, gp], "alog": ext["alog"][j, gp], "dsk": ext["dsk"][j, gp],
                          "ngb": ext["sngb"][j, gp], "U": ext["U"], "ones64": ext["ones64"], "I64": ext["I64"],
                          "NEGrep": ext["NEGrep"], "identb": ext["identb"]}
                emit_ssm_core(p, S, gp=gp)
                p.end_phase()
            KC, OT, w_out = 16, OT16, ext["ssm_w_out"][j]
        p.bind = {"x": xcur, "OT": OT, "w_out": w_out, "g1b": modv[l, 2], "y": xb}
        emit_outproj(p, T, KC)
        p.end_phase()
        final = (l == L - 1)
        p.bind = {"x": xb, "a2": fmv(l, 3), "sh2": fmv(l, 4), "g2b": modv[l, 5], "ident": ext["ident"],
                  "y": out if final else xa}
        if l % 2 == 0:
            p.bind.update({"wg": ext["ffn_wg"][j], "wu": ext["ffn_wu"][j], "wd": ext["ffn_wd"][j]})
            emit_ffn(p, T, 1, False, final)
        else:
            p.bind.update({"wg": ext["moe_wg"][j], "wu": ext["moe_wu"][j], "wd": ext["moe_wd"][j], "wr": ext["wr"][j],
                           "fgb": ext["fgb"]})
            emit_ffn(p, T, 8, True, final)
        xcur = xa
        if not final:
            p.end_phase()
    return p.finish()


def _fm(v):
    return np.ascontiguousarray(np.asarray(v).reshape(8, 128).T)


def _bc(v, n):
    v = np.asarray(v)
    return np.ascontiguousarray(np.broadcast_to(v, (n,) + v.shape))


def kernel(x, c, positions, ada_w, ada_b, norm_g,
           mla_w_in, mla_q_norm, mla_kv_norm, mla_w_uq, mla_w_ukv, mla_w_out,
           ssm_w_in, ssm_conv_w, ssm_conv_b, ssm_dt_bias, ssm_a_log, ssm_d, ssm_norm, ssm_w_out,
           ffn_w_gate, ffn_w_up, ffn_w_down,
           moe_w_router, moe_w_gate, moe_w_up, moe_w_down, final_norm):
    f32 = np.float32
    A = lambda a: np.ascontiguousarray(np.asarray(a))
    x = A(x).astype(f32, copy=False)
    pos = A(positions).astype(np.int32, copy=False)
    nc = build_fused(S_, L_)
    bf = mybir.dt.np_dtype(BF16) if hasattr(mybir.dt, "np_dtype") else None
    if bf is None:
        import ml_dtypes
        bf = ml_dtypes.bfloat16
    cwj, cbj = A(ssm_conv_w), A(ssm_conv_b)
    cw = np.empty((2, 2, 128, 12, 4), f32)
    cb = np.empty((2, 2, 128, 12), f32)
    for j in range(2):
        for gp in range(2):
            chs = list(range(gp * 8, gp * 8 + 8)) + [16 + 2 * gp, 17 + 2 * gp, 20 + 2 * gp, 21 + 2 * gp]
            cidx = np.array(chs)[:, None] * 128 + np.arange(128)[None, :]
            cw[j, gp] = cwj[j][:, cidx].transpose(2, 1, 0)
            cb[j, gp] = cbj[j][cidx].T
    hs = lambda v, n: np.ascontiguousarray(np.stack([np.stack([np.broadcast_to(A(v)[j][gp * 16:(gp + 1) * 16], (n, 16))
                                                                for gp in range(2)]) for j in range(2)]))
    sngb = np.ascontiguousarray(np.stack([np.stack([np.broadcast_to(A(ssm_norm)[j][gp * 1024:(gp + 1) * 1024], (64, 1024))
                                                    for gp in range(2)]) for j in range(2)]))
    shared = {
        "ada_w": A(ada_w), "ada_b": np.ascontiguousarray(np.broadcast_to(A(ada_b)[:, None, :], (L_, 128, 6 * D))),
        "ngb4": np.ascontiguousarray(np.broadcast_to(A(norm_g)[:, :, None, :], (L_, 2, 128, D))),
        "ones32": np.ones((128, 128), f32), "invf": rope_consts(), "ident": np.eye(128, dtype=f32),
        "onesbf": np.ones((128, 64), bf), "identb": np.eye(128).astype(bf),
        "mla_w_in": A(mla_w_in), "qn": np.ascontiguousarray(A(mla_q_norm).reshape(2, 3, 128).transpose(0, 2, 1)),
        "kvn": np.ascontiguousarray(A(mla_kv_norm).reshape(2, 2, 128).transpose(0, 2, 1)),
        "mla_w_uq": A(mla_w_uq), "mla_w_ukv": A(mla_w_ukv), "mla_w_out": A(mla_w_out),
        "ffn_wg": A(ffn_w_gate)[:, None], "ffn_wu": A(ffn_w_up)[:, None], "ffn_wd": A(ffn_w_down)[:, None],
        "ssm_w_in": A(ssm_w_in), "cw": cw, "cb": cb, "dtb": hs(ssm_dt_bias, 64), "alog": hs(ssm_a_log, 128),
        "dsk": hs(ssm_d, 64), "sngb": sngb, "ssm_w_out": A(ssm_w_out), "wr": A(moe_w_router),
        "moe_wg": A(moe_w_gate), "moe_wu": A(moe_w_up), "moe_wd": A(moe_w_down), "fgb": _bc(A(final_norm), 128),
    }
    shared.update(ssm_consts())
    maps = []
    for b in range(B_):
        m = dict(shared)
        m["x"] = np.ascontiguousarray(x[b])
        m["c"] = _fm(A(c)[b])
        m["posb"] = _bc(pos[b], 128)
        maps.append(m)
    res = run_bass_kernel_spmd(nc, maps, core_ids=list(range(B_)))
    return np.stack([np.asarray(res.results[b]["out"]) for b in range(B_)], axis=0).astype(f32, copy=False)
```

```python
import contextlib
import numpy as np
import concourse.bass as bass
import concourse.mybir as mybir
from concourse.bass_utils import run_bass_kernel_spmd

F32 = mybir.dt.float32
BF16 = mybir.dt.bfloat16
I32 = mybir.dt.int32
AF = mybir.ActivationFunctionType
ALU = mybir.AluOpType
AX = mybir.AxisListType

D = 1024
NCORES = 8
EPS = 1e-6

ENGS = ("pe", "act", "dve", "pool", "sp")


class _Op:
    __slots__ = ("eng", "fn", "is_dma", "is_mm", "waits", "need_inc", "count", "dsem", "dval")

    def __init__(self, eng, fn, is_dma, is_mm):
        self.eng = eng
        self.fn = fn
        self.is_dma = is_dma
        self.is_mm = is_mm
        self.waits = []
        self.need_inc = False
        self.count = 0
        self.dsem = None
        self.dval = 0


class Sched:
    def __init__(self, nc, dma_slots=8):
        self.nc = nc
        self.dma_slots = dma_slots
        self.dma_n = {e: 0 for e in ENGS}
        self.cnt = {e: 0 for e in ENGS}
        self.out_dmas = []
        self.st = contextlib.ExitStack()
        self.csem = {e: self.st.enter_context(nc.semaphore(f"c_{e}")) for e in ENGS}
        self.dsem = {}
        for e in ("sp", "act", "pool"):
            for s_ in range(dma_slots):
                self.dsem[(e, s_)] = self.st.enter_context(nc.semaphore(f"d_{e}{s_}"))
        self.barrier = {}
        self.seen = {e: {} for e in ENGS}
        self._reset()

    def _reset(self):
        self.ops = {e: [] for e in ENGS}
        self.last_w = {}
        self.readers = {}

    def add(self, eng, fn, R=(), W=(), dma=False, mm=False, is_out=False):
        op = _Op(eng, fn, dma, mm)
        deps = []
        for r in R:
            w = self.last_w.get(r)
            if w is not None:
                deps.append(w)
        for w_ in W:
            w = self.last_w.get(w_)
            if w is not None:
                deps.append(w)
            deps.extend(self.readers.get(w_, ()))
        seen = set()
        for d in deps:
            if id(d) in seen or d is op:
                continue
            seen.add(id(d))
            if d.eng == eng and not d.is_dma and not dma and eng == "pe" and d.is_mm and mm:
                continue
            op.waits.append(d)
        for r in R:
            self.readers.setdefault(r, []).append(op)
        for w_ in W:
            self.last_w[w_] = op
            self.readers[w_] = []
        self.ops[eng].append(op)
        if dma:
            n = self.dma_n[eng]
            self.dma_n[eng] = n + 1
            op.dsem = (eng, n % self.dma_slots)
            op.dval = 16 * (n // self.dma_slots + 1)
            if is_out:
                self.out_dmas.append(op)
        return op

    def emit_phase(self, last=False):
        nc = self.nc
        for e in ENGS:
            for op in self.ops[e]:
                for d in op.waits:
                    if not d.is_dma:
                        d.need_inc = True
            comp = [op for op in self.ops[e] if not op.is_dma]
            if comp:
                comp[-1].need_inc = True
        for e in ENGS:
            c = self.cnt[e]
            for op in self.ops[e]:
                if not op.is_dma and op.need_inc:
                    c += 1
                    op.count = c
            self.cnt[e] = c
        csem, dsem = self.csem, self.dsem
        barrier = dict(self.barrier)
        ops = self.ops
        out_dmas = self.out_dmas
        seen_all = self.seen

        def run(e, eng):
            seen = seen_all[e]
            for key, val in barrier.items():
                if key == ("c", e) or seen.get(key, 0) >= val:
                    continue
                seen[key] = val
                eng.wait_ge(dsem[key[1]] if key[0] == "d" else csem[key[1]], val)
            for op in ops[e]:
                need = {}
                for d in op.waits:
                    if d.is_dma:
                        key = ("d", d.dsem)
                        val = d.dval
                    else:
                        key = ("c", d.eng)
                        val = d.count
                    if need.get(key, 0) < val:
                        need[key] = val
                if op.is_dma and op.dval > 16:
                    key = ("d", op.dsem)
                    if need.get(key, 0) < op.dval - 16:
                        need[key] = op.dval - 16
                for key, val in need.items():
                    if seen.get(key, 0) >= val:
                        continue
                    seen[key] = val
                    eng.wait_ge(dsem[key[1]] if key[0] == "d" else csem[key[1]], val)
                inst = op.fn(eng)
                if op.is_dma:
                    inst.then_inc(dsem[op.dsem], 16)
                elif op.need_inc:
                    inst.then_inc(csem[e], 1)
            if last and e == "sp":
                fin = {}
                for op in out_dmas:
                    if fin.get(op.dsem, 0) < op.dval:
                        fin[op.dsem] = op.dval
                for k, v in fin.items():
                    eng.wait_ge(dsem[k], v)

        with nc.Block() as block:
            @block.tensor
            def _(eng):
                run("pe", eng)

            @block.scalar
            def _(eng):
                run("act", eng)

            @block.vector
            def _(eng):
                run("dve", eng)

            @block.gpsimd
            def _(eng):
                run("pool", eng)

            @block.sync
            def _(eng):
                run("sp", eng)
        for e in ENGS:
            if self.cnt[e]:
                self.barrier[("c", e)] = self.cnt[e]
            n = self.dma_n[e]
            for s_ in range(min(self.dma_slots, n)):
                last_n = n - 1 - ((n - 1 - s_) % self.dma_slots)
                self.barrier[("d", (e, s_))] = 16 * (last_n // self.dma_slots + 1)
        self._reset()

    def close(self):
        self.st.close()


class Prog:
    def __init__(self):
        self.nc = bass.Bass("TRN2", target_bir_lowering=False)
        self.S = Sched(self.nc)
        self.st = contextlib.ExitStack()
        self.outs = []
        self.bind = {}
        self.nphase = 0

    def din(self, name, shape, dt=F32):
        if name in self.bind:
            return self.bind[name]
        return self.nc.dram_tensor(name, list(shape), dt, kind="ExternalInput").ap()

    def dout(self, name, shape, dt=F32):
        if name in self.bind:
            return self.bind[name]
        self.outs.append(name)
        return self.nc.dram_tensor(name, list(shape), dt, kind="ExternalOutput").ap()

    def scratch(self, name, shape, dt=F32):
        return self.nc.dram_tensor(name, list(shape), dt, kind="Internal").ap()

    def sb(self, name, shape, dt=F32):
        return self.st.enter_context(self.nc.sbuf_tensor(f"s{self.nphase}_" + name, list(shape), dt))

    def ps(self, name, shape=(128, 512), dt=F32):
        return self.st.enter_context(self.nc.psum_tensor(f"p{self.nphase}_" + name, list(shape), dt))

    def end_phase(self, last=False):
        self.S.emit_phase(last=last)
        self.st.close()
        self.st = contextlib.ExitStack()
        self.nphase += 1
        self.bind = {}

    def dma(self, q, out, in_, R=(), W=(), is_out=False, slow=False):
        kw = {"allow_slow_non_contiguous": True}
        return self.S.add(q, lambda e: e.dma_start(out=out, in_=in_, **kw), R, W, dma=True, is_out=is_out)

    def mm(self, out, lhsT, rhs, start, stop, R=(), W=()):
        return self.S.add("pe", lambda e: e.matmul(out, lhsT, rhs, start=start, stop=stop), R, W, mm=True)

    def tr(self, out, in_, ident, R=(), W=()):
        return self.S.add("pe", lambda e: e.transpose(out, in_, ident), R, W, mm=True)

    def act(self, out, in_, func, bias=None, scale=None, accum_out=None, R=(), W=()):
        kw = {}
        if bias is not None:
            kw["bias"] = bias
        if scale is not None:
            kw["scale"] = scale
        if accum_out is not None:
            kw["accum_out"] = accum_out
        return self.S.add("act", lambda e: e.activation(out, in_, func, **kw), R, W)

    def ts(self, eng, out, in0, s1, s2, op0, op1=None, R=(), W=()):
        if op1 is None:
            return self.S.add(eng, lambda e: e.tensor_scalar(out, in0, s1, None, op0), R, W)
        return self.S.add(eng, lambda e: e.tensor_scalar(out, in0, s1, s2, op0, op1), R, W)

    def tt(self, eng, out, in0, in1, op, R=(), W=()):
        return self.S.add(eng, lambda e: e.tensor_tensor(out, in0, in1, op), R, W)

    def stt(self, out, in0, scalar, in1, op0, op1, R=(), W=()):
        return self.S.add("dve", lambda e: e.scalar_tensor_tensor(out, in0, scalar, in1, op0, op1), R, W)

    def red(self, out, in_, op, R=(), W=()):
        return self.S.add("dve", lambda e: e.tensor_reduce(out, in_, AX.X, op), R, W)

    def recip(self, out, in_, R=(), W=()):
        return self.S.add("dve", lambda e: e.reciprocal(out, in_), R, W)

    def copy(self, eng, out, in_, R=(), W=()):
        return self.S.add(eng, lambda e: e.tensor_copy(out, in_), R, W)

    def memset(self, eng, ap, val, W=()):
        return self.S.add(eng, lambda e: e.memset(ap, val), (), W)

    def finish(self):
        self.end_phase(last=True)
        self.S.close()
        return self.nc


def emit_norm_tile(p, c, xt, xt_tok, ti, hT, hT_tokbase, h32=None):
    k = c["nt"]
    c["nt"] = k + 1
    b = k % 2
    xn = c["xn"][b]
    ss = c["ss"][b]
    R, Wt = [xt_tok], [f"xn{b}", f"ss{b}"]
    p.act(xn[:, :], xt[:, :], AF.Square, accum_out=ss[:, 0:1], R=R, W=Wt)
    p.ts("dve", ss[:, 1:2], ss[:, 0:1], 1.0 / D, EPS, ALU.mult, ALU.add, R=[f"ss{b}"], W=[f"ss{b}"])
    p.act(ss[:, 2:3], ss[:, 1:2], AF.Sqrt, R=[f"ss{b}"], W=[f"ss{b}"])
    p.recip(ss[:, 3:4], ss[:, 2:3], R=[f"ss{b}"], W=[f"ss{b}"])
    p.ts("dve", xn[:, :], xt[:, :], ss[:, 3:4], None, ALU.mult, R=[xt_tok, f"ss{b}"], W=[f"xn{b}"])
    for half in range(2):
        bank = c["tp"][half]
        btok = c["tp_tok"][half]
        for q in range(4):
            kc = half * 4 + q
            p.tr(bank[:, q * 128:(q + 1) * 128], xn[:, kc * 128:(kc + 1) * 128], c["ident"][:, :],
                 R=[f"xn{b}", "ident"], W=[btok])
        for q in range(4):
            kc = half * 4 + q
            p.act(hT[:, kc, ti * 128:(ti + 1) * 128], bank[:, q * 128:(q + 1) * 128], AF.Identity,
                  bias=c["sh"][:, kc:kc + 1], scale=c["a"][:, kc:kc + 1],
                  R=[btok, "modv"], W=[f"{hT_tokbase}.{ti}.{kc}"])
            if h32 is not None:
                p.act(h32[:, kc, :], bank[:, q * 128:(q + 1) * 128], AF.Identity,
                      bias=c["sh"][:, kc:kc + 1], scale=c["a"][:, kc:kc + 1],
                      R=[btok, "modv"], W=[f"h32.{kc}"])


FF = 2816
NFF = FF // 128
UNITS = [(0, 3), (3, 3), (6, 3), (9, 3), (12, 3), (15, 3), (18, 2), (20, 2)]


def build_ffn(T, E, route, final, dbg=0, **kw):
    p = Prog()
    emit_ffn(p, T, E, route, final, dbg, **kw)
    return p.finish()


def emit_ffn(p, T, E, route, final, dbg=0, **kw):
    x_in = p.din("x", [T, D])
    a_in = p.din("a2", [128, 8])
    sh_in = p.din("sh2", [128, 8])
    g2b_in = p.din("g2b", [128, D])
    wg = p.din("wg", [E, D, FF])
    wu = p.din("wu", [E, D, FF])
    wd = p.din("wd", [E, FF, D])
    ident_in = p.din("ident", [128, 128])
    if route:
        wr_in = p.din("wr", [D, 8])
    if final:
        fg_in = p.din("fgb", [128, D])
    y_out = p.dout("y", [T, D])

    SG = min(T, 2048)
    NSG = T // SG
    NT = SG // 128
    NG = SG // 512

    ident = p.sb("ident", [128, 128])
    a_sb = p.sb("a_sb", [128, 8])
    sh_sb = p.sb("sh_sb", [128, 8])
    g2b = p.sb("g2b_sb", [128, D])
    hT = p.sb("hT", [128, 8, SG], BF16)
    acc = p.sb("acc", [128, NT, D])
    xt = [p.sb(f"xt{i}", [128, D]) for i in range(2)]
    xn = [p.sb(f"xn{i}", [128, D]) for i in range(2)]
    ss = [p.sb(f"ss{i}", [128, 4]) for i in range(2)]
    wgs = [p.sb(f"wg{i}", [128, 8, 384], BF16) for i in range(2)]
    wus = [p.sb(f"wu{i}", [128, 8, 384], BF16) for i in range(2)]
    wds = [p.sb(f"wd{i}", [128, 3, D], BF16) for i in range(2)]
    aT = [p.sb(f"aT{i}", [128, 3, 512], BF16) for i in range(2)]
    sl = [p.sb(f"sl{i}", [128, 512]) for i in range(2)]
    gates = p.sb("gates", [128, NT, 8])
    if route:
        wr = p.sb("wr_sb", [128, 8, 8])
        h32 = p.sb("h32", [128, 8, 128])
        gsc = p.sb("gsc", [128, 48])
    if final:
        fgb = p.sb("fgb_sb", [128, D])
    banks = [p.ps(f"bank{i}") for i in range(8)]

    p.dma("sp", ident[:, :], ident_in, W=["ident"])
    p.dma("sp", a_sb[:, :], a_in, W=["modv"])
    p.dma("sp", sh_sb[:, :], sh_in, W=["modv"])
    p.dma("sp", g2b[:, :], g2b_in, W=["g2b"])
    if route:
        p.dma("sp", wr[:, :, :], wr_in.rearrange("(kc p) e -> p kc e", p=128), W=["wr"])
    if final:
        p.dma("sp", fgb[:, :], fg_in, W=["fgb"])

    c = {"nt": 0, "xn": xn, "ss": ss, "tp": [banks[0], banks[1]], "tp_tok": ["bank0", "bank1"],
         "ident": ident, "a": a_sb, "sh": sh_sb}

    nunit = 0
    nld = 0
    for sg in range(NSG):
        tok0 = sg * SG
        for ti in range(NT):
            b = nld % 2
            nld += 1
            p.dma("sp", xt[b][:, :], x_in[tok0 + ti * 128: tok0 + (ti + 1) * 128, :], W=[f"xt{b}"])
            emit_norm_tile(p, c, xt[b], f"xt{b}", ti, hT, "hT", h32 if route else None)
            if route:
                lg_ps = banks[2]
                for kc in range(8):
                    p.mm(lg_ps[:, 0:8], h32[:, kc, :], wr[:, kc, :], kc == 0, kc == 7,
                         R=[f"h32.{kc}", "wr"], W=["bank2"])
                G = ["gsc"]
                lg = gsc[:, 0:8]
                m1 = gsc[:, 8:9]
                mk1 = gsc[:, 16:24]
                l2 = gsc[:, 24:32]
                m2 = gsc[:, 9:10]
                mk2 = gsc[:, 32:40]
                dd = gsc[:, 10:11]
                ex = gsc[:, 11:12]
                g1 = gsc[:, 12:13]
                g2_ = gsc[:, 13:14]
                gt = gsc[:, 40:48]
                p.copy("dve", lg, lg_ps[:, 0:8], R=["bank2"], W=G)
                p.red(m1, lg, ALU.max, R=G, W=G)
                p.ts("dve", mk1, lg, m1, None, ALU.is_equal, R=G, W=G)
                p.stt(l2, mk1, -1e30, lg, ALU.mult, ALU.add, R=G, W=G)
                p.red(m2, l2, ALU.max, R=G, W=G)
                p.ts("dve", mk2, l2, m2, None, ALU.is_equal, R=G, W=G)
                p.tt("dve", dd, m2, m1, ALU.subtract, R=G, W=G)
                p.act(ex, dd, AF.Exp, R=G, W=G)
                p.ts("dve", g1, ex, 1.0, None, ALU.add, R=G, W=G)
                p.recip(g1, g1, R=G, W=G)
                p.tt("dve", g2_, ex, g1, ALU.mult, R=G, W=G)
                p.ts("dve", gt, mk1, g1, None, ALU.mult, R=G, W=G)
                p.stt(gates[:, ti, :], mk2, g2_, gt, ALU.mult, ALU.add, R=G, W=[f"gates.{ti}"])
        for e in range(E if dbg != 1 else 0):
            for (j0, nf) in UNITS:
                first_acc = (e == 0 and j0 == 0)
                wb = nunit % 2
                nunit += 1
                wtok = f"w{wb}"
                p.dma("pool", wgs[wb][:, :, 0:nf * 128],
                      wg[e].rearrange("(kc p) f -> p kc f", p=128)[:, :, j0 * 128:(j0 + nf) * 128], W=[wtok + "g"])
                p.dma("pool", wus[wb][:, :, 0:nf * 128],
                      wu[e].rearrange("(kc p) f -> p kc f", p=128)[:, :, j0 * 128:(j0 + nf) * 128], W=[wtok + "u"])
                p.dma("pool", wds[wb][:, 0:nf, :],
                      wd[e].rearrange("(j p) d -> p j d", p=128)[:, j0:j0 + nf, :], W=[wtok + "d"])
                abase = c.setdefault("nab", 0)
                c["nab"] = abase + NG

                def gu_(g, e=e, nf=nf, wb=wb, wtok=wtok, abase=abase):
                    ab = (abase + g) % 2
                    for j in range(nf):
                        k = c.setdefault("ngu", 0)
                        c["ngu"] = k + 1
                        gb = banks[2 + (k % 2)]
                        ub = banks[4 + (k % 2)]
                        gtok, utok = f"bank{2 + k % 2}", f"bank{4 + k % 2}"
                        for kc in range(8):
                            p.mm(gb[:, :], wgs[wb][:, kc, j * 128:(j + 1) * 128], hT[:, kc, g * 512:(g + 1) * 512],
                                 kc == 0, kc == 7,
                                 R=[wtok + "g"] + [f"hT.{g * 4 + t}.{kc}" for t in range(4)], W=[gtok])
                        for kc in range(8):
                            p.mm(ub[:, :], wus[wb][:, kc, j * 128:(j + 1) * 128], hT[:, kc, g * 512:(g + 1) * 512],
                                 kc == 0, kc == 7,
                                 R=[wtok + "u"] + [f"hT.{g * 4 + t}.{kc}" for t in range(4)], W=[utok])
                        sb_ = k % 2
                        p.act(sl[sb_][:, :], gb[:, :], AF.Silu, R=[gtok], W=[f"sl{sb_}"])
                        p.tt("dve", aT[ab][:, j, :], sl[sb_][:, :], ub[:, :], ALU.mult,
                             R=[f"sl{sb_}", utok], W=[f"aT{ab}.{j}"])

                def dn_(g, e=e, nf=nf, wb=wb, wtok=wtok, abase=abase, first_acc=first_acc):
                    ab = (abase + g) % 2
                    for t in range(4):
                        ti = g * 4 + t
                        for half in range(2):
                            k = c.setdefault("ndn", 0)
                            c["ndn"] = k + 1
                            db = banks[6 + (k % 2)]
                            dtok = f"bank{6 + k % 2}"
                            for j in range(nf):
                                p.mm(db[:, :], aT[ab][:, j, t * 128:(t + 1) * 128],
                                     wds[wb][:, j, half * 512:(half + 1) * 512], j == 0, j == nf - 1,
                                     R=[f"aT{ab}.{j}", wtok + "d"], W=[dtok])
                            dst = acc[:, ti, half * 512:(half + 1) * 512]
                            atok = f"acc.{ti}.{half}"
                            if route:
                                gsl = gates[:, ti, e:e + 1]
                                if first_acc:
                                    p.ts("dve", dst, db[:, :], gsl, None, ALU.mult,
                                         R=[dtok, f"gates.{ti}"], W=[atok])
                                else:
                                    p.stt(dst, db[:, :], gsl, dst, ALU.mult, ALU.add,
                                          R=[dtok, f"gates.{ti}", atok], W=[atok])
                            else:
                                if first_acc:
                                    p.copy("dve", dst, db[:, :], R=[dtok], W=[atok])
                                else:
                                    p.tt("dve", dst, db[:, :], dst, ALU.add, R=[dtok, atok], W=[atok])

                gu_(0)
                for g in range(NG):
                    if g + 1 < NG:
                        gu_(g + 1)
                    dn_(g)
        for ti in range(NT):
            b = nld % 2
            nld += 1
            p.dma("sp", xt[b][:, :], x_in[tok0 + ti * 128: tok0 + (ti + 1) * 128, :], W=[f"xt{b}"])
            at = [f"acc.{ti}.0", f"acc.{ti}.1"]
            p.tt("dve", acc[:, ti, :], acc[:, ti, :], g2b[:, :], ALU.mult, R=at + ["g2b"], W=at)
            p.tt("pool", acc[:, ti, :], acc[:, ti, :], xt[b][:, :], ALU.add, R=at + [f"xt{b}"], W=at)
            if final:
                k = c["nt"]
                c["nt"] = k + 1
                sb_ = k % 2
                s_ = ss[sb_]
                st = f"ss{sb_}"
                p.act(xn[sb_][:, :], acc[:, ti, :], AF.Square, accum_out=s_[:, 0:1], R=at, W=[f"xn{sb_}", st])
                p.ts("dve", s_[:, 1:2], s_[:, 0:1], 1.0 / D, EPS, ALU.mult, ALU.add, R=[st], W=[st])
                p.act(s_[:, 2:3], s_[:, 1:2], AF.Sqrt, R=[st], W=[st])
                p.recip(s_[:, 3:4], s_[:, 2:3], R=[st], W=[st])
                p.stt(acc[:, ti, :], acc[:, ti, :], s_[:, 3:4], fgb[:, :], ALU.mult, ALU.mult,
                      R=at + [st, "fgb"], W=at)
            p.dma("sp", y_out[tok0 + ti * 128: tok0 + (ti + 1) * 128, :], acc[:, ti, :], R=at, is_out=True)


TWO_PI = float(2.0 * np.pi)
C1 = 6.28125
C2 = float(2.0 * np.pi - 6.28125)
PI32 = float(np.float32(np.pi))


def build_mod(T, L=4, **kw):
    p = Prog()
    emit_mod(p, T, L, **kw)
    return p.finish()


def emit_mod(p, T, L=4, **kw):
    c_in = p.din("c", [128, 8])
    adaw = p.din("ada_w", [L, D, 6 * D])
    adab = p.din("ada_b", [L, 128, 6 * D])
    ngb = p.din("ngb", [L, 2, 128, D])
    ones_in = p.din("ones", [128, 128])
    posb = p.din("posb", [128, T], I32)
    invf = p.din("invf", [128, 2])
    modv = p.dout("modv", [L, 6, 128, D])
    cos_o = p.dout("cos2", [128, T])
    sin_o = p.dout("sin2s", [128, T])

    ones = p.sb("ones", [128, 128])
    cond = p.sb("cond", [128, 8])
    crep = p.sb("crep", [128, 8, 128])
    wt = [p.sb(f"wt{i}", [128, 8, 512]) for i in range(2)]
    bb = p.sb("bb", [128, 6 * D])
    modb = p.sb("modb", [128, 6 * D])
    ng = p.sb("ng", [128, 2, D])
    ao = [p.sb(f"ao{i}", [128, D]) for i in range(2)]
    banks = [p.ps(f"bank{i}") for i in range(2)]

    p.dma("sp", ones[:, :], ones_in, W=["ones"])
    p.dma("sp", cond[:, :], c_in, W=["cond"])
    p.act(cond[:, :], cond[:, :], AF.Silu, R=["cond"], W=["cond"])
    for kc in range(8):
        p.ts("dve", crep[:, kc, :], ones[:, :], cond[:, kc:kc + 1], None, ALU.mult, R=["ones", "cond"], W=["crep"])
    nw = 0
    for l in range(L):
        p.dma("act", bb[:, :], adab[l], W=["bb"])
        p.dma("act", ng[:, :, :], ngb[l].rearrange("t p d -> p t d"), W=["ng"])
        for n in range(12):
            b = nw % 2
            nw += 1
            p.dma("sp", wt[b][:, :, :], adaw[l].rearrange("(kc p) n -> p kc n", p=128)[:, :, n * 512:(n + 1) * 512],
                  W=[f"wt{b}"])
            for kc in range(8):
                p.mm(banks[b][:, :], crep[:, kc, :], wt[b][:, kc, :], kc == 0, kc == 7,
                     R=["crep", f"wt{b}"], W=[f"bank{b}"])
            p.tt("dve", modb[:, n * 512:(n + 1) * 512], banks[b][:, :], bb[:, n * 512:(n + 1) * 512], ALU.add,
                 R=[f"bank{b}", "bb"], W=[f"modb{n // 2}"])
        for s in range(2):
            p.stt(ao[s][:, :], modb[:, (3 * s + 1) * D:(3 * s + 2) * D], 1.0, ng[:, s, :], ALU.add, ALU.mult,
                  R=[f"modb{3 * s + 1}", "ng"], W=[f"ao{s}"])
            p.dma("act", modv[l, 3 * s + 0], ao[s][:, :], R=[f"ao{s}"], is_out=True)
            p.dma("act", modv[l, 3 * s + 1], modb[:, (3 * s) * D:(3 * s + 1) * D], R=[f"modb{3 * s}"], is_out=True)
            p.dma("act", modv[l, 3 * s + 2], modb[:, (3 * s + 2) * D:(3 * s + 3) * D], R=[f"modb{3 * s + 2}"],
                  is_out=True)

    CH = min(T, 2048)
    fq = p.sb("fq", [128, 2])
    pi_ = p.sb("pi", [128, CH], I32)
    ang = p.sb("ang", [128, CH])
    kr = p.sb("kr", [128, CH])
    ki = p.sb("ki", [128, CH], I32)
    r = p.sb("r", [128, CH])
    m = p.sb("m", [128, CH])
    so = p.sb("so", [128, CH])
    p.dma("sp", fq[:, :], invf, W=["fq"])
    for ch in range(T // CH):
        sl_ = slice(ch * CH, (ch + 1) * CH)
        p.dma("sp", pi_[:, :], posb[:, sl_], W=["pi"])
        p.copy("dve", ang[:, :], pi_[:, :], R=["pi"], W=["ang"])
        p.ts("dve", ang[:, :], ang[:, :], fq[:, 0:1], None, ALU.mult, R=["ang", "fq"], W=["ang"])
        p.ts("dve", kr[:, :], ang[:, :], 1.0 / TWO_PI, None, ALU.mult, R=["ang"], W=["kr"])
        p.copy("dve", ki[:, :], kr[:, :], R=["kr"], W=["ki"])
        p.copy("dve", kr[:, :], ki[:, :], R=["ki"], W=["kr"])
        p.stt(r[:, :], kr[:, :], -C1, ang[:, :], ALU.mult, ALU.add, R=["kr", "ang"], W=["r"])
        p.stt(r[:, :], kr[:, :], -C2, r[:, :], ALU.mult, ALU.add, R=["kr", "r"], W=["r"])
        p.ts("dve", m[:, :], r[:, :], PI32, None, ALU.is_gt, R=["r"], W=["m"])
        p.stt(r[:, :], m[:, :], -TWO_PI, r[:, :], ALU.mult, ALU.add, R=["m", "r"], W=["r"])
        p.ts("dve", m[:, :], r[:, :], -PI32, None, ALU.is_lt, R=["r"], W=["m"])
        p.stt(r[:, :], m[:, :], TWO_PI, r[:, :], ALU.mult, ALU.add, R=["m", "r"], W=["r"])
        p.act(so[:, :], r[:, :], AF.Sin, R=["r"], W=["so"])
        p.ts("dve", so[:, :], so[:, :], fq[:, 1:2], None, ALU.mult, R=["so", "fq"], W=["so"])
        p.dma("sp", sin_o[:, sl_], so[:, :], R=["so"], is_out=True)
        p.ts("dve", r[:, :], r[:, :], float(np.pi / 2), None, ALU.add, R=["r"], W=["r"])
        p.ts("dve", m[:, :], r[:, :], PI32, None, ALU.is_gt, R=["r"], W=["m"])
        p.stt(r[:, :], m[:, :], -TWO_PI, r[:, :], ALU.mult, ALU.add, R=["m", "r"], W=["r"])
        p.act(so[:, :], r[:, :], AF.Sin, R=["r"], W=["so"])
        p.dma("sp", cos_o[:, sl_], so[:, :], R=["so"], is_out=True)


def rope_consts():
    inv = (10000.0 ** (-np.arange(0, 32, 2, dtype=np.float32) / 32)).astype(np.float32)
    t = np.zeros((128, 2), np.float32)
    for r_ in range(128):
        j = r_ % 32
        t[r_, 0] = inv[j % 16]
        t[r_, 1] = -1.0 if j < 16 else 1.0
    return t


def build_mla_pre(T, **kw):
    p = Prog()
    emit_mla_pre(p, T, **kw)
    return p.finish()


def emit_mla_pre(p, T, **kw):
    x_in = p.din("x", [T, D])
    a_in = p.din("a1", [128, 8])
    sh_in = p.din("sh1", [128, 8])
    win_d = p.din("w_in", [D, 672])
    qn_in = p.din("qn", [128, 3])
    kvn_in = p.din("kvn", [128, 2])
    wuq_d = p.din("w_uq", [384, 1536])
    wukv_d = p.din("w_ukv", [256, 2048])
    cos_d = p.din("cos2", [128, T])
    sin_d = p.din("sin2s", [128, T])
    ident_in = p.din("ident", [128, 128])
    QN = p.dout("QN", [8, 128, T], BF16)
    QR = p.dout("QR", [4, 128, T], BF16)
    KN = p.dout("KN", [8, 128, T], BF16)
    KR = p.dout("KR", [32, T], BF16)
    V = p.dout("V", [T, D], BF16)

    NG = T // 512
    ident = p.sb("ident", [128, 128])
    identb = p.sb("identb", [128, 128], BF16)
    a_sb = p.sb("a_sb", [128, 8])
    sh_sb = p.sb("sh_sb", [128, 8])
    gn = p.sb("gn", [128, 5])
    win = p.sb("win", [128, 8, 672], BF16)
    winS = p.sb("winS", [128, 8, 32], BF16)
    wqn = p.sb("wqn", [128, 3, 1024], BF16)
    wqr = p.sb("wqr", [128, 3, 512], BF16)
    wqs = p.sb("wqs", [128, 3, 512], BF16)
    wkn = p.sb("wkn", [128, 2, 1024], BF16)
    wv = p.sb("wv", [128, 2, 1024], BF16)
    hT = [p.sb(f"hT{i}", [128, 8, 512], BF16) for i in range(2)]
    cT = [p.sb(f"cT{i}", [128, 5, 512], BF16) for i in range(2)]
    xt = [p.sb(f"xt{i}", [128, D]) for i in range(2)]
    xn = [p.sb(f"xn{i}", [128, D]) for i in range(2)]
    ss = [p.sb(f"ss{i}", [128, 4]) for i in range(2)]
    s2 = [p.sb(f"s2{i}", [128, 8]) for i in range(2)]
    junk = p.sb("junk", [128, 384])
    cqk = [p.sb(f"cqk{i}", [128, 640], BF16) for i in range(2)]
    cs = [p.sb(f"cs{i}", [128, 512]) for i in range(2)]
    sn = [p.sb(f"sn{i}", [128, 512]) for i in range(2)]
    t1 = [p.sb(f"t1{i}", [128, 512]) for i in range(2)]
    t2 = [p.sb(f"t2{i}", [128, 512]) for i in range(2)]
    stg = [p.sb(f"stg{i}", [128, 512], BF16) for i in range(4)]
    banks = [p.ps(f"bank{i}") for i in range(4)] + [None] + [p.ps(f"bank{i}") for i in range(5, 8)]
    trb = p.ps("trb", [128, 1024], BF16)

    p.dma("sp", ident[:, :], ident_in, W=["ident"])
    p.copy("dve", identb[:, :], ident[:, :], R=["ident"], W=["identb"])
    p.dma("sp", a_sb[:, :], a_in, W=["modv"])
    p.dma("sp", sh_sb[:, :], sh_in, W=["modv"])
    p.dma("sp", gn[:, 0:3], qn_in, W=["gn"])
    p.dma("sp", gn[:, 3:5], kvn_in, W=["gn"])
    p.dma("pool", win[:, :, :], win_d.rearrange("(kc p) f -> p kc f", p=128), W=["win"])
    p.dma("pool", winS[:, :, 0:16], win_d.rearrange("(kc p) f -> p kc f", p=128)[:, :, 656:672], W=["winS"])
    p.dma("pool", winS[:, :, 16:32], win_d.rearrange("(kc p) f -> p kc f", p=128)[:, :, 640:656], W=["winS"])
    uq4 = wuq_d.rearrange("(kc p) (h d) -> p kc h d", p=128, d=96)
    ukv4 = wukv_d.rearrange("(kc p) (h d) -> p kc h d", p=128, d=128)
    for kc in range(3):
        p.dma("pool", wqn[:, kc, :].rearrange("p (h d) -> p h d", d=64), uq4[:, kc, :, 0:64], W=["wqn"])
        p.dma("pool", wqr[:, kc, :].rearrange("p (h d) -> p h d", d=32), uq4[:, kc, :, 64:96], W=["wqr"])
        p.dma("pool", wqs[:, kc, :].rearrange("p (h d) -> p h d", d=32)[:, :, 0:16], uq4[:, kc, :, 80:96], W=["wqs"])
        p.dma("pool", wqs[:, kc, :].rearrange("p (h d) -> p h d", d=32)[:, :, 16:32], uq4[:, kc, :, 64:80], W=["wqs"])
    for kc in range(2):
        p.dma("pool", wkn[:, kc, :].rearrange("p (h d) -> p h d", d=64), ukv4[:, kc, :, 0:64], W=["wkn"])
        p.dma("pool", wv[:, kc, :].rearrange("p (h d) -> p h d", d=64), ukv4[:, kc, :, 64:128], W=["wv"])

    c = {"nt": 0, "xn": xn, "ss": ss, "tp": [banks[0], banks[1]], "tp_tok": ["bank0", "bank1"],
         "ident": ident, "a": a_sb, "sh": sh_sb}
    cnt = {"ob": 0, "stg": 0, "ld": 0}

    def obank():
        k = cnt["ob"]
        cnt["ob"] = k + 1
        i = 5 + k % 3
        return banks[i], f"bank{i}"

    def stage():
        k = cnt["stg"]
        cnt["stg"] = k + 1
        return stg[k % 4], f"stg{k % 4}", k

    for g in range(NG):
        hb = g % 2
        h_ = hT[hb]
        c_ = cT[hb]
        tok0 = g * 512
        p.dma("act", cs[hb][:, :], cos_d[:, tok0:tok0 + 512], W=[f"cs{hb}"])
        p.dma("act", sn[hb][:, :], sin_d[:, tok0:tok0 + 512], W=[f"sn{hb}"])
        for t in range(4):
            b = cnt["ld"] % 2
            cnt["ld"] += 1
            p.dma("sp", xt[b][:, :], x_in[tok0 + t * 128: tok0 + (t + 1) * 128, :], W=[f"xt{b}"])
            emit_norm_tile(p, c, xt[b], f"xt{b}", t, h_, f"hT{hb}")
            hR = [f"hT{hb}.{t}.{kc}" for kc in range(8)]
            for kc in range(8):
                p.mm(banks[2][:, 0:384], h_[:, kc, t * 128:(t + 1) * 128], win[:, kc, 0:384], kc == 0, kc == 7,
                     R=hR + ["win"], W=["bank2"])
            for kc in range(8):
                p.mm(banks[3][:, 0:256], h_[:, kc, t * 128:(t + 1) * 128], win[:, kc, 384:640], kc == 0, kc == 7,
                     R=hR + ["win"], W=["bank3"])
            sb_ = b
            s_ = s2[sb_]
            st = f"s2{sb_}"
            p.act(junk[:, 0:384], banks[2][:, 0:384], AF.Square, scale=float(384 ** -0.5), accum_out=s_[:, 0:1],
                  R=["bank2"], W=["junk", st])
            p.act(junk[:, 0:256], banks[3][:, 0:256], AF.Square, scale=float(256 ** -0.5), accum_out=s_[:, 1:2],
                  R=["bank3"], W=["junk", st])
            p.ts("dve", s_[:, 2:4], s_[:, 0:2], EPS, None, ALU.add, R=[st], W=[st])
            p.act(s_[:, 4:6], s_[:, 2:4], AF.Sqrt, R=[st], W=[st])
            p.recip(s_[:, 6:8], s_[:, 4:6], R=[st], W=[st])
            cq = cqk[sb_]
            ctok = f"cqk{sb_}"
            p.ts("dve", cq[:, 0:384], banks[2][:, 0:384], s_[:, 6:7], None, ALU.mult, R=["bank2", st], W=[ctok])
            p.ts("dve", cq[:, 384:640], banks[3][:, 0:256], s_[:, 7:8], None, ALU.mult, R=["bank3", st], W=[ctok])
            for q in range(5):
                p.tr(trb[:, q * 128:(q + 1) * 128], cq[:, q * 128:(q + 1) * 128], identb[:, :],
                     R=[ctok, "identb"], W=["trb"])
            for q in range(5):
                p.act(c_[:, q, t * 128:(t + 1) * 128], trb[:, q * 128:(q + 1) * 128], AF.Copy,
                      scale=gn[:, q:q + 1], R=["trb", "gn"], W=[f"cT{hb}.{t}"])
        cR = [f"cT{hb}.{t}" for t in range(4)]
        hRall = [f"hT{hb}.{t}.{kc}" for t in range(4) for kc in range(8)]
        for which, wsb, wtok, nk, koff, dst in (("qn", wqn, "wqn", 3, 0, QN), ("kn", wkn, "wkn", 2, 3, KN)):
            for ch in range(8):
                bk, btok = obank()
                for kc in range(nk):
                    p.mm(bk[:, :], wsb[:, kc, ch * 128:(ch + 1) * 128], c_[:, koff + kc, :], kc == 0, kc == nk - 1,
                         R=cR + [wtok], W=[btok])
                sg, stok, k = stage()
                if k % 2 == 0:
                    p.act(sg[:, :], bk[:, :], AF.Copy, R=[btok], W=[stok])
                else:
                    p.copy("dve", sg[:, :], bk[:, :], R=[btok], W=[stok])
                p.dma("sp", dst[ch, :, tok0:tok0 + 512], sg[:, :], R=[stok], is_out=True)
        for t in range(4):
            for half in range(2):
                bk, btok = obank()
                for kc in range(2):
                    p.mm(bk[:, :], c_[:, 3 + kc, t * 128:(t + 1) * 128], wv[:, kc, half * 512:(half + 1) * 512],
                         kc == 0, kc == 1, R=[f"cT{hb}.{t}", "wv"], W=[btok])
                sg, stok, k = stage()
                if k % 2 == 0:
                    p.act(sg[:, :], bk[:, :], AF.Copy, R=[btok], W=[stok])
                else:
                    p.copy("dve", sg[:, :], bk[:, :], R=[btok], W=[stok])
                p.dma("sp", V[tok0 + t * 128: tok0 + (t + 1) * 128, half * 512:(half + 1) * 512], sg[:, :],
                      R=[stok], is_out=True)
        jobs = [(wqr[:, :, ch * 128:(ch + 1) * 128], wqs[:, :, ch * 128:(ch + 1) * 128], 3, 0, c_, cR, 128,
                 QR[ch, :, tok0:tok0 + 512], ["wqr", "wqs"]) for ch in range(4)]
        jobs.append((win[:, :, 640:672], winS[:, :, :], 8, 0, h_, hRall, 32, KR[:, tok0:tok0 + 512], ["win", "winS"]))
        for (wa, ws_, nk, koff, src, sR, M, dst, wR) in jobs:
            b1, b1t = obank()
            for kc in range(nk):
                p.mm(b1[0:M, :], wa[:, kc, :], src[:, koff + kc, :], kc == 0, kc == nk - 1, R=sR + wR, W=[b1t])
            b2, b2t = obank()
            for kc in range(nk):
                p.mm(b2[0:M, :], ws_[:, kc, :], src[:, koff + kc, :], kc == 0, kc == nk - 1, R=sR + wR, W=[b2t])
            k = cnt.setdefault("rp", 0)
            cnt["rp"] = k + 1
            tb = k % 2
            p.tt("dve", t1[tb][0:M, :], b1[0:M, :], cs[hb][0:M, :], ALU.mult, R=[b1t, f"cs{hb}"], W=[f"t1{tb}"])
            p.tt("dve", t2[tb][0:M, :], b2[0:M, :], sn[hb][0:M, :], ALU.mult, R=[b2t, f"sn{hb}"], W=[f"t2{tb}"])
            sg, stok, _ = stage()
            p.tt("pool", sg[0:M, :], t1[tb][0:M, :], t2[tb][0:M, :], ALU.add, R=[f"t1{tb}", f"t2{tb}"], W=[stok])
            p.dma("sp", dst, sg[0:M, :], R=[stok], is_out=True)


def build_attn(S, NH, **kw):
    p = Prog()
    emit_attn(p, S, NH, **kw)
    return p.finish()


def emit_attn(p, S, NH, src=None, **kw):
    if src is None:
        QT = p.din("QT", [NH, 96, S], BF16)
        KT = p.din("KT", [NH, 96, S], BF16)
        Vd = p.din("V", [NH, 128, (S // 128) * 64], BF16)
    ones_in = p.din("ones", [128, 64], BF16)
    OT = p.dout("OT", [NH // 2, 128, S], BF16)
    NKT = S // 128
    NG = S // 512
    scale = float(96 ** -0.5)

    ones = p.sb("ones", [128, 64], BF16)
    q_sb = [p.sb(f"q{i}", [96, S], BF16) for i in range(2)]
    k_sb = [p.sb(f"k{i}", [96, S], BF16) for i in range(2)]
    v_sb = [p.sb(f"v{i}", [128, NKT * 64], BF16) for i in range(2)]
    NB = 4
    LA = 3
    pT = [p.sb(f"pT{i}", [128, 512], BF16) for i in range(NB)]
    rl = [p.sb(f"rl{i}", [64, 512]) for i in range(2)]
    og = [p.sb(f"og{i}", [64, 512], BF16) for i in range(2)]
    sbk = [p.ps(f"sb{i}") for i in range(NB)]
    obk = [p.ps(f"ob{i}") for i in range(2)]
    lbk = [p.ps(f"lb{i}") for i in range(2)]
    p.dma("sp", ones[:, :], ones_in, W=["ones"])

    def load_head(h):
        hb = h % 2
        if src is None:
            p.dma("sp", q_sb[hb][:, :], QT[h], W=[f"q{hb}"])
            p.dma("sp", k_sb[hb][:, :], KT[h], W=[f"k{hb}"])
            p.dma("sp", v_sb[hb][:, :], Vd[h], W=[f"v{hb}"])
        else:
            r2 = slice((h % 2) * 64, (h % 2) * 64 + 64)
            r4 = slice((h % 4) * 32, (h % 4) * 32 + 32)
            p.dma("sp", q_sb[hb][0:64, :], src["QN"][h // 2, r2, :], W=[f"q{hb}"])
            p.dma("sp", q_sb[hb][64:96, :], src["QR"][h // 4, r4, :], W=[f"q{hb}"])
            p.dma("sp", k_sb[hb][0:64, :], src["KN"][h // 2, r2, :], W=[f"k{hb}"])
            p.dma("sp", k_sb[hb][64:96, :], src["KR"], W=[f"k{hb}"])
            vsrc = src["V"].rearrange("(kt p) d -> p kt d", p=128)
            KQ = min(8, NKT)
            for kq in range(0, NKT, KQ):
                p.dma("act", v_sb[hb][:, kq * 64:(kq + KQ) * 64].rearrange("p (kt d) -> p kt d", d=64),
                      vsrc[:, kq:kq + KQ, h * 64:(h + 1) * 64], W=[f"v{hb}"])

    its = [(h, g, kt) for h in range(NH) for g in range(NG) for kt in range(4 * (g + 1))]
    N = len(its)
    load_head(0)
    for n in range(N + LA):
        if n < N:
            h, g, kt = its[n]
            hb = h % 2
            sb_ = n % NB
            p.mm(sbk[sb_][:, :], k_sb[hb][:, kt * 128:(kt + 1) * 128], q_sb[hb][:, g * 512:(g + 1) * 512],
                 True, True, R=[f"k{hb}", f"q{hb}"], W=[f"sb{sb_}"])
            p.act(pT[sb_][:, :], sbk[sb_][:, :], AF.Exp, scale=scale, R=[f"sb{sb_}"], W=[f"pT{sb_}"])
            i = kt - 4 * g
            if i >= 0:
                if i > 0:
                    p.memset("pool", pT[sb_][0:64, 0:128 * i], 0.0, W=[f"pT{sb_}"])
                p.memset("pool", pT[sb_][64:128, 0:128 * i + 64], 0.0, W=[f"pT{sb_}"])
        m = n - LA
        if m >= 0:
            h, g, kt = its[m]
            hb = h % 2
            gb = (h * NG + g) % 2
            nkt = 4 * (g + 1)
            sb_ = m % NB
            p.mm(obk[gb][0:64, :], v_sb[hb][:, kt * 64:(kt + 1) * 64], pT[sb_][:, :], kt == 0, kt == nkt - 1,
                 R=[f"v{hb}", f"pT{sb_}"], W=[f"ob{gb}"])
            p.mm(lbk[gb][0:64, :], ones[:, :], pT[sb_][:, :], kt == 0, kt == nkt - 1,
                 R=["ones", f"pT{sb_}"], W=[f"lb{gb}"])
            if g == 0 and kt == 0 and h + 1 < NH:
                load_head(h + 1)
            if kt == nkt - 1:
                p.recip(rl[gb][:, :], lbk[gb][0:64, :], R=[f"lb{gb}"], W=[f"rl{gb}"])
                p.tt("dve", og[gb][:, :], obk[gb][0:64, :], rl[gb][:, :], ALU.mult, R=[f"ob{gb}", f"rl{gb}"],
                     W=[f"og{gb}"])
                p.dma("act", OT[h // 2, (h % 2) * 64:(h % 2) * 64 + 64, g * 512:(g + 1) * 512], og[gb][:, :],
                      R=[f"og{gb}"], is_out=True)


def build_outproj(T, KC, **kw):
    p = Prog()
    emit_outproj(p, T, KC, **kw)
    return p.finish()


def emit_outproj(p, T, KC, **kw):
    x_in = p.din("x", [T, D])
    ot_in = p.din("OT", [KC, 128, T], BF16)
    w_in = p.din("w_out", [KC * 128, D])
    g1b_in = p.din("g1b", [128, D])
    y_out = p.dout("y", [T, D])
    w = p.sb("w", [128, KC, D], BF16)
    g1b = p.sb("g1b", [128, D])
    ot = [p.sb(f"ot{i}", [128, KC, 512], BF16) for i in range(2)]
    xt = [p.sb(f"xt{i}", [128, D]) for i in range(3)]
    tmp = [p.sb(f"tmp{i}", [128, 512]) for i in range(2)]
    banks = [p.ps(f"bank{i}") for i in range(4)]
    wv_ = w_in.rearrange("(kc p) d -> p kc d", p=128)
    for k0 in range(0, KC, 4):
        p.dma("pool", w[:, k0:k0 + 4, :], wv_[:, k0:k0 + 4, :], W=["w"])
    p.dma("sp", g1b[:, :], g1b_in, W=["g1b"])
    n = 0
    nx = 0
    for g in range(T // 512):
        gb = g % 2
        p.dma("act", ot[gb][:, :, :], ot_in[:, :, g * 512:(g + 1) * 512].rearrange("k p t -> p k t"), W=[f"ot{gb}"])
        for t in range(4):
            xb = nx % 3
            nx += 1
            r0 = g * 512 + t * 128
            p.dma("sp", xt[xb][:, :], x_in[r0:r0 + 128, :], W=[f"xt{xb}"])
            for half in range(2):
                bi = n % 4
                tb = n % 2
                n += 1
                for kc in range(KC):
                    p.mm(banks[bi][:, :], ot[gb][:, kc, t * 128:(t + 1) * 128], w[:, kc, half * 512:(half + 1) * 512],
                         kc == 0, kc == KC - 1, R=[f"ot{gb}", "w"], W=[f"bank{bi}"])
                p.tt("dve", tmp[tb][:, :], banks[bi][:, :], g1b[:, half * 512:(half + 1) * 512], ALU.mult,
                     R=[f"bank{bi}", "g1b"], W=[f"tmp{tb}"])
                p.tt("pool", xt[xb][:, half * 512:(half + 1) * 512], xt[xb][:, half * 512:(half + 1) * 512],
                     tmp[tb][:, :], ALU.add, R=[f"tmp{tb}", f"xt{xb}"], W=[f"xt{xb}"])
            p.dma("sp", y_out[r0:r0 + 128, :], xt[xb][:, :], R=[f"xt{xb}"], is_out=True)


SSM_IN = 5152


def build_ssm_pre(T, **kw):
    p = Prog()
    emit_ssm_pre(p, T, **kw)
    return p.finish()


def emit_ssm_pre(p, T, **kw):
    x_in = p.din("x", [T, D])
    a_in = p.din("a1", [128, 8])
    sh_in = p.din("sh1", [128, 8])
    w_d = p.din("w_in", [D, SSM_IN])
    ident_in = p.din("ident", [128, 128])
    fused = "PT24" in p.bind
    if fused:
        PT24 = p.bind["PT24"]
        ZT = p.bind["ZT"]
        DTt = p.bind["DTt"]
    else:
        PT = p.dout("PT", [40, 128, T], BF16)
        DT = p.dout("DT", [32, T])
    ident = p.sb("ident", [128, 128])
    a_sb = p.sb("a_sb", [128, 8])
    sh_sb = p.sb("sh_sb", [128, 8])
    w = p.sb("w", [128, 8, SSM_IN], BF16)
    hT = [p.sb(f"hT{i}", [128, 8, 512], BF16) for i in range(2)]
    xt = [p.sb(f"xt{i}", [128, D]) for i in range(2)]
    xn = [p.sb(f"xn{i}", [128, D]) for i in range(2)]
    ss = [p.sb(f"ss{i}", [128, 4]) for i in range(2)]
    stg = [p.sb(f"stg{i}", [128, 512], BF16) for i in range(4)]
    stf = p.sb("stf", [32, 512])
    stf2 = p.sb("stf2", [128, 32])
    banks = [p.ps(f"bank{i}") for i in range(6)]
    p.dma("sp", ident[:, :], ident_in, W=["ident"])
    p.dma("sp", a_sb[:, :], a_in, W=["modv"])
    p.dma("sp", sh_sb[:, :], sh_in, W=["modv"])
    wv_ = w_d.rearrange("(kc p) f -> p kc f", p=128)
    for kc in range(8):
        p.dma("pool", w[:, kc, :], wv_[:, kc, :], W=[f"w{kc}"])
    c = {"nt": 0, "xn": xn, "ss": ss, "tp": [banks[0], banks[1]], "tp_tok": ["bank0", "bank1"],
         "ident": ident, "a": a_sb, "sh": sh_sb}
    n = 0
    nld = 0
    for g in range(T // 512):
        hb = g % 2
        tok0 = g * 512
        for t in range(4):
            b = nld % 2
            nld += 1
            p.dma("sp", xt[b][:, :], x_in[tok0 + t * 128: tok0 + (t + 1) * 128, :], W=[f"xt{b}"])
            emit_norm_tile(p, c, xt[b], f"xt{b}", t, hT[hb], f"hT{hb}")
        hR = [f"hT{hb}.{t}.{kc}" for t in range(4) for kc in range(8)]
        if fused:
            for t in range(4):
                hRt = [f"hT{hb}.{t}.{kc}" for kc in range(8)]
                r0 = tok0 + t * 128
                for nq in range(5):
                    N = 512 if nq < 4 else 32
                    c0 = nq * 512 if nq < 4 else 5120
                    bi = 2 + n % 4
                    si = n % 4
                    n += 1
                    for kc in range(8):
                        p.mm(banks[bi][:, 0:N], hT[hb][:, kc, t * 128:(t + 1) * 128], w[:, kc, c0:c0 + N],
                             kc == 0, kc == 7, R=hRt + [f"w{kc}"], W=[f"bank{bi}"])
                    if nq < 4:
                        if n % 2 == 0:
                            p.act(stg[si][:, :], banks[bi][:, :], AF.Copy, R=[f"bank{bi}"], W=[f"stg{si}"])
                        else:
                            p.copy("dve", stg[si][:, :], banks[bi][:, :], R=[f"bank{bi}"], W=[f"stg{si}"])
                        p.dma("sp", ZT[r0:r0 + 128, c0:c0 + 512], stg[si][:, :], R=[f"stg{si}"], W=["ZT"])
                    else:
                        p.copy("dve", stf2[:, :], banks[bi][:, 0:32], R=[f"bank{bi}"], W=["stf2"])
                        p.dma("sp", DTt[r0:r0 + 128, :], stf2[:, :], R=["stf2"], W=["DTt"])
        for ch in (range(16, 40) if fused else range(41)):
            M = 128 if ch < 40 else 32
            bi = 2 + n % 4
            si = n % 4
            n += 1
            for kc in range(8):
                p.mm(banks[bi][0:M, :], w[:, kc, ch * 128: ch * 128 + M], hT[hb][:, kc, :], kc == 0, kc == 7,
                     R=hR + [f"w{kc}"], W=[f"bank{bi}"])
            if ch < 40:
                if n % 2 == 0:
                    p.act(stg[si][:, :], banks[bi][:, :], AF.Copy, R=[f"bank{bi}"], W=[f"stg{si}"])
                else:
                    p.copy("dve", stg[si][:, :], banks[bi][:, :], R=[f"bank{bi}"], W=[f"stg{si}"])
                if fused:
                    p.dma("sp", PT24[ch - 16, :, tok0:tok0 + 512], stg[si][:, :], R=[f"stg{si}"], W=["PT24"])
                else:
                    p.dma("sp", PT[ch, :, tok0:tok0 + 512], stg[si][:, :], R=[f"stg{si}"], is_out=True)
            else:
                p.copy("dve", stf[:, :], banks[bi][0:32, :], R=[f"bank{bi}"], W=["stf"])
                p.dma("sp", DT[:, tok0:tok0 + 512], stf[:, :], R=["stf"], is_out=True)


def build_ssm_core(S, **kw):
    p = Prog()
    emit_ssm_core(p, S, **kw)
    return p.finish()


def emit_ssm_core(p, S, **kw):
    NCH = S // 64
    fused = "PT24" in p.bind
    gp = kw.get("gp", 0)
    if fused:
        PT24 = p.bind["PT24"]
        zt = p.bind["ZT"][:, gp * 1024:(gp + 1) * 1024]
        dtr_d = p.bind["DTt"][:, gp * 16:(gp + 1) * 16].rearrange("(c l) h -> l c h", l=64)
        OTf = p.bind["OTf"]
    else:
        uT = p.din("uT", [12, 128, S + 3], BF16)
        zt = p.din("zt", [S, 1024], BF16)
        dtr_d = p.din("dtr", [64, NCH, 16])
    cw_d = p.din("cw", [128, 12, 4])
    cb_d = p.din("cb", [128, 12])
    dtb_d = p.din("dtb", [64, 16])
    alog_d = p.din("alog", [128, 16])
    dsk_d = p.din("dsk", [64, 16])
    ngb_d = p.din("ngb", [64, 1024])
    U_d = p.din("U", [64, 64])
    ones_d = p.din("ones64", [64, 128])
    I_d = p.din("I64", [64, 64])
    neg_d = p.din("NEGrep", [64, 512])
    idb_d = p.din("identb", [128, 128], BF16)
    if not fused:
        Y = p.dout("Y", [S, 1024], BF16)

    cw = p.sb("cw", [128, 12, 4])
    cb = p.sb("cb", [128, 12])
    dtb = p.sb("dtb", [64, 16])
    aB = p.sb("aB", [128, 16])
    dsk = p.sb("dsk", [64, 16])
    ngb = p.sb("ngb", [64, 1024])
    U = p.sb("U", [64, 64])
    ones64 = p.sb("ones64", [64, 128])
    I64 = p.sb("I64", [64, 64])
    NEG = p.sb("NEG", [64, 512])
    identb = p.sb("identb", [128, 128], BF16)
    dt_all = p.sb("dt_all", [64, NCH, 16])
    dtA_all = p.sb("dtA_all", [64, NCH, 16])
    ub = [p.sb(f"ub{i}", [128, 12, 515], BF16) for i in range(2)]
    xsT = [p.sb(f"xsT{i}", [128, 12, 512], BF16) for i in range(2)]
    acc = [p.sb(f"acc{i}", [128, 512]) for i in range(2)]
    xs = [p.sb(f"xs{i}", [64, 1024], BF16) for i in range(2)]
    Bt = [p.sb(f"Bt{i}", [64, 256], BF16) for i in range(2)]
    zb = [p.sb(f"zb{i}", [64, 1024], BF16) for i in range(2)]
    zs = p.sb("zs", [64, 1024])
    sm2 = [p.sb(f"sm{i}", [128, 96]) for i in range(2)]
    MTf = [p.sb(f"MTf{i}", [64, 1024], BF16) for i in range(2)]
    xdt2 = [p.sb(f"xdt{i}", [64, 1024], BF16) for i in range(2)]
    xdte2 = [p.sb(f"xdte{i}", [64, 1024], BF16) for i in range(2)]
    t22 = [p.sb(f"t2{i}", [64, 1024]) for i in range(2)]
    R = p.sb("R", [64, 1024])
    segm = p.sb("segm", [64, 512])
    E = p.sb("E", [64, 512])
    Gs = p.sb("Gs", [64, 128])
    yf = p.sb("yf", [64, 1024])
    S32 = p.sb("S32", [128, 1024])
    Sb = p.sb("Sb", [128, 1024], BF16)
    junk = p.sb("junk", [64, 512])
    gst = p.sb("gst", [64, 8])
    Yst = [p.sb(f"Yst{i}", [64, 1024], BF16) for i in range(2)]
    YTs = [p.sb(f"YTs{i}", [128, 512], BF16) for i in range(2)]
    trx = p.ps("trx", [128, 1024], BF16)
    trB = p.ps("trB", [128, 1024], BF16)
    smp = p.ps("smp")
    gbk = p.ps("gbk")
    segb = p.ps("segb")
    ydb = p.ps("ydb")
    yob = p.ps("yob")
    stb = p.ps("stb")

    for (dst, src, tok) in ((cw[:, :, :], cw_d, "cw"), (cb[:, :], cb_d, "cb"), (dtb[:, :], dtb_d, "dtb"),
                            (aB[:, :], alog_d, "aB"), (dsk[:, :], dsk_d, "dsk"), (ngb[:, :], ngb_d, "ngb"),
                            (U[:, :], U_d, "U"), (ones64[:, :], ones_d, "ones64"), (I64[:, :], I_d, "I64"),
                            (NEG[:, :], neg_d, "NEG"), (identb[:, :], idb_d, "identb")):
        p.dma("sp", dst, src, W=[tok])
    for c0 in range(0, NCH, 8):
        p.dma("sp", dt_all[:, c0:c0 + 8, :], dtr_d[:, c0:c0 + 8, :], W=["dt_all"])
    p.act(aB[:, :], aB[:, :], AF.Exp, R=["aB"], W=["aB"])
    p.ts("dve", aB[:, :], aB[:, :], -1.0, None, ALU.mult, R=["aB"], W=["aB"])
    p.tt("dve", dt_all[:, :, :], dt_all[:, :, :], dtb[:, :].unsqueeze(1).broadcast_to([64, NCH, 16]), ALU.add,
         R=["dt_all", "dtb"], W=["dt_all"])
    p.act(dt_all[:, :, :], dt_all[:, :, :], AF.Exp, R=["dt_all"], W=["dt_all"])
    p.act(dt_all[:, :, :], dt_all[:, :, :], AF.Ln, bias=1.0, R=["dt_all"], W=["dt_all"])
    p.tt("dve", dtA_all[:, :, :], dt_all[:, :, :], aB[0:64, :].unsqueeze(1).broadcast_to([64, NCH, 16]), ALU.mult,
         R=["dt_all", "aB"], W=["dtA_all"])
    p.memset("dve", S32[:, :], 0.0, W=["S32"])
    p.memset("pool", Sb[:, :], 0.0, W=["Sb"])

    def b3(ap, n):
        return ap.unsqueeze(2).broadcast_to([64, n, 64])

    nacc = [0]

    def conv_block(blk):
        bb = blk % 2
        tok0 = blk * 512
        if not fused:
            p.dma("sp", ub[bb][:, :, :], uT[:, :, tok0:tok0 + 515].rearrange("j p t -> p j t"), W=[f"ub{bb}"])
        else:
            lo = 3 if blk == 0 else 0
            if blk == 0:
                p.memset("pool", ub[bb][:, :, 0:3], 0.0, W=[f"ub{bb}"])
            for (d0, nchk, s0) in ((0, 8, gp * 8), (8, 2, 16 + 2 * gp), (10, 2, 20 + 2 * gp)):
                p.dma("sp", ub[bb][:, d0:d0 + nchk, lo:515],
                      PT24[s0:s0 + nchk, :, tok0 - 3 + lo:tok0 + 512].rearrange("j p t -> p j t"), W=[f"ub{bb}"])
        for j in range(12):
            ab = nacc[0] % 2
            nacc[0] += 1
            a_ = acc[ab]
            at = f"acc{ab}"
            p.ts("dve", a_[:, :], ub[bb][:, j, 0:512], cw[:, j, 0:1], None, ALU.mult, R=[f"ub{bb}", "cw"], W=[at])
            for k in range(1, 4):
                p.stt(a_[:, :], ub[bb][:, j, k:k + 512], cw[:, j, k:k + 1], a_[:, :], ALU.mult, ALU.add,
                      R=[f"ub{bb}", "cw", at], W=[at])
            p.act(xsT[bb][:, j, :], a_[:, :], AF.Silu, bias=cb[:, j:j + 1], R=[at, "cb"], W=[f"xsT{bb}.{j}"])

    def front(c):
        blk, cc = divmod(c, 8)
        bb = blk % 2
        o = cc * 64
        pp = c % 2
        sm = sm2[pp]
        smt = f"sm{pp}"
        p.dma("act", zb[pp][:, :], zt[c * 64:(c + 1) * 64, :], W=[f"zb{pp}"])
        for j in range(8):
            p.tr(trx[0:64, j * 128:(j + 1) * 128], xsT[bb][:, j, o:o + 64], identb[:, :],
                 R=[f"xsT{bb}.{j}", "identb"], W=["trx"])
        for j in range(2):
            p.tr(trB[0:64, j * 128:(j + 1) * 128], xsT[bb][:, 8 + j, o:o + 64], identb[:, :],
                 R=[f"xsT{bb}.{8 + j}", "identb"], W=["trB"])
        p.act(xs[pp][:, :], trx[0:64, :], AF.Copy, R=["trx"], W=[f"xs{pp}"])
        p.copy("dve", Bt[pp][:, :], trB[0:64, 0:256], R=["trB"], W=[f"Bt{pp}"])
        yield
        dtA_c = dtA_all[:, c, :]
        dt_c = dt_all[:, c, :]
        p.mm(smp[0:64, 0:16], U[:, :], dtA_c, True, True, R=["U", "dtA_all"], W=["smp"])
        p.mm(smp[:, 16:32], ones64[:, :], dtA_c, True, True, R=["ones64", "dtA_all"], W=["smp"])
        acs = sm[0:64, 0:16]
        nacs = sm[0:64, 16:32]
        ea = sm[0:64, 32:48]
        dte = sm[0:64, 48:64]
        cdB = sm[:, 64:80]
        p.copy("dve", acs, smp[0:64, 0:16], R=["smp"], W=[smt + ".acs"])
        p.ts("dve", nacs, acs, -1.0, None, ALU.mult, R=[smt + ".acs"], W=[smt + ".nacs"])
        p.act(ea, acs, AF.Exp, R=[smt + ".acs"], W=[smt + ".ea"])
        p.tt("dve", dte, smp[0:64, 16:32], acs, ALU.subtract, R=["smp", smt + ".acs"], W=[smt + ".dte"])
        p.act(dte, dte, AF.Exp, R=[smt + ".dte"], W=[smt + ".dte"])
        p.act(cdB, smp[:, 16:32], AF.Exp, R=["smp"], W=[smt + ".cd"])
        yield
        p.tt("dve", R[:, :].rearrange("p (h l) -> p h l", l=64), b3(dtA_c, 16),
             U[:, :].unsqueeze(1).broadcast_to([64, 16, 64]), ALU.mult, R=["dtA_all", "U"], W=["R"])
        for lg in range(2):
            p.mm(gbk[0:64, lg * 64:(lg + 1) * 64], xsT[bb][:, 8 + lg, o:o + 64], xsT[bb][:, 10 + lg, o:o + 64],
                 True, True, R=[f"xsT{bb}.{8 + lg}", f"xsT{bb}.{10 + lg}"], W=["gbk"])
        p.copy("dve", Gs[:, :], gbk[0:64, 0:128], R=["gbk"], W=["Gs"])
        yield
        v3 = lambda t_: t_[:, :].rearrange("p (h d) -> p h d", d=64)
        p.tt("pool", v3(xdt2[pp]), v3(xs[pp]), b3(dt_c, 16), ALU.mult, R=[f"xs{pp}", "dt_all"], W=[f"xdt{pp}"])
        p.tt("pool", v3(xdte2[pp]), v3(xdt2[pp]), b3(dte, 16), ALU.mult, R=[f"xdt{pp}", smt + ".dte"], W=[f"xdte{pp}"])
        p.tt("pool", v3(t22[pp]), v3(xs[pp]), b3(dsk[:, :], 16), ALU.mult, R=[f"xs{pp}", "dsk"], W=[f"t2{pp}"])
        yield
        for hf in range(2):
            cs_ = slice(hf * 512, hf * 512 + 512)
            p.mm(segb[0:64, :], ones64[:, 0:64], R[:, cs_], True, False, R=["ones64", "R"], W=["segb"])
            p.mm(segb[0:64, :], I64[:, :], NEG[:, :], False, True, R=["I64", "NEG"], W=["segb"])
            p.tt("dve", segm[:, :].rearrange("p (h l) -> p h l", l=64),
                 segb[0:64, :].rearrange("p (h l) -> p h l", l=64), b3(sm[0:64, 16 + hf * 8:16 + hf * 8 + 8], 8),
                 ALU.add, R=["segb", smt + ".nacs"], W=["segm"])
            p.act(E[:, :], segm[:, :], AF.Exp, R=["segm"], W=["E"])
            yield
            p.tt("dve", MTf[pp][:, cs_].rearrange("p (h l) -> p h l", l=64), E[:, :].rearrange("p (h l) -> p h l", l=64),
                 Gs[:, hf * 64:(hf + 1) * 64].unsqueeze(1).broadcast_to([64, 8, 64]), ALU.mult,
                 R=["E", "Gs"], W=[f"MT{pp}.{hf}"])
            yield

    def back(c):
        blk, cc = divmod(c, 8)
        bb = blk % 2
        o = cc * 64
        pp = c % 2
        sm = sm2[pp]
        smt = f"sm{pp}"
        for hf in range(2):
            cs_ = slice(hf * 512, hf * 512 + 512)
            for hh in range(8):
                h = hf * 8 + hh
                p.mm(ydb[0:64, hh * 64:(hh + 1) * 64], MTf[pp][:, h * 64:(h + 1) * 64], xdt2[pp][:, h * 64:(h + 1) * 64],
                     True, True, R=[f"MT{pp}.{hf}", f"xdt{pp}"], W=["ydb"])
            for hh in range(8):
                h = hf * 8 + hh
                p.mm(yob[0:64, hh * 64:(hh + 1) * 64], xsT[bb][:, 10 + hf, o:o + 64], Sb[:, h * 64:(h + 1) * 64],
                     True, True, R=[f"xsT{bb}.{10 + hf}", f"Sb{hf}"], W=["yob"])
            for hh in range(8):
                h = hf * 8 + hh
                p.mm(stb[:, hh * 64:(hh + 1) * 64], Bt[pp][:, hf * 128:(hf + 1) * 128], xdte2[pp][:, h * 64:(h + 1) * 64],
                     True, True, R=[f"Bt{pp}", f"xdte{pp}"], W=["stb"])
            yield
            yh = yf[:, cs_]
            p.tt("dve", yh.rearrange("p (h d) -> p h d", d=64), yob[0:64, :].rearrange("p (h d) -> p h d", d=64),
                 b3(sm[0:64, 32 + hf * 8:32 + hf * 8 + 8], 8), ALU.mult, R=["yob", smt + ".ea"], W=[f"yf{hf}"])
            p.tt("dve", yh, yh, ydb[0:64, :], ALU.add, R=["ydb", f"yf{hf}"], W=[f"yf{hf}"])
            p.tt("pool", yh, yh, t22[pp][:, cs_], ALU.add, R=[f"t2{pp}", f"yf{hf}"], W=[f"yf{hf}"])
            yield
            Sh = S32[:, cs_]
            p.tt("pool", Sh.rearrange("p (h d) -> p h d", d=64), Sh.rearrange("p (h d) -> p h d", d=64),
                 sm[:, 64 + hf * 8:64 + hf * 8 + 8].unsqueeze(2).broadcast_to([128, 8, 64]), ALU.mult,
                 R=[f"S32{hf}", smt + ".cd"], W=[f"S32{hf}"])
            p.tt("dve", Sh, Sh, stb[:, :], ALU.add, R=["stb", f"S32{hf}"], W=[f"S32{hf}"])
            p.copy("pool", Sb[:, cs_], Sh, R=[f"S32{hf}"], W=[f"Sb{hf}"])
            yield
        p.act(zs[:, :], zb[pp][:, :], AF.Silu, R=[f"zb{pp}"], W=["zs"])
        p.tt("dve", yf[:, :], yf[:, :], zs[:, :], ALU.mult, R=["yf0", "yf1", "zs"], W=["yf0", "yf1"])
        yield
        for lg in range(2):
            p.act(junk[:, :], yf[:, lg * 512:(lg + 1) * 512], AF.Square, scale=float(512 ** -0.5),
                  accum_out=gst[:, lg:lg + 1], R=[f"yf{lg}"], W=["junk", "gst"])
        p.ts("dve", gst[:, 2:4], gst[:, 0:2], EPS, None, ALU.add, R=["gst"], W=["gst"])
        p.act(gst[:, 4:6], gst[:, 2:4], AF.Sqrt, R=["gst"], W=["gst"])
        p.recip(gst[:, 6:8], gst[:, 4:6], R=["gst"], W=["gst"])
        yield
        for lg in range(2):
            cs_ = slice(lg * 512, (lg + 1) * 512)
            p.stt(Yst[pp][:, cs_], yf[:, cs_], gst[:, 6 + lg:7 + lg], ngb[:, cs_], ALU.mult, ALU.mult,
                  R=[f"yf{lg}", "gst", "ngb"], W=[f"Yst{pp}"])
        if not fused:
            p.dma("sp", Y[c * 64:(c + 1) * 64, :], Yst[pp][:, :], R=[f"Yst{pp}"], is_out=True)
        else:
            for j in range(8):
                p.tr(trB[:, 256 + j * 64:256 + (j + 1) * 64], Yst[pp][:, j * 128:(j + 1) * 128], identb[0:64, 0:64],
                     R=[f"Yst{pp}", "identb"], W=["trB"])
            p.act(YTs[pp][:, :], trB[:, 256:768], AF.Copy, R=["trB"], W=[f"YTs{pp}"])
            p.dma("sp", OTf[gp * 8:(gp + 1) * 8, :, c * 64:(c + 1) * 64].rearrange("j p t -> p j t"),
                  YTs[pp][:, :].rearrange("p (j t) -> p j t", t=64), R=[f"YTs{pp}"], W=["OTf"])

    for c in range(NCH + 1):
        gens = []
        if c < NCH:
            if c % 8 == 0:
                conv_block(c // 8)
            gens.append(front(c))
        if c >= 1:
            gens.append(back(c - 1))
        while gens:
            for g_ in list(gens):
                try:
                    next(g_)
                except StopIteration:
                    gens.remove(g_)


def ssm_consts():
    k = np.arange(64)
    U = (k[:, None] <= k[None, :]).astype(np.float32)
    NEG = np.where(k[:, None] > k[None, :], -30000.0, 0.0).astype(np.float32)
    return {"U": U, "ones64": np.ones((64, 128), np.float32), "I64": np.eye(64, dtype=np.float32),
            "NEGrep": np.ascontiguousarray(np.tile(NEG, (1, 8)))}


B_, S_ = 4, 8192
L_ = 4


def build_fused(S=S_, L=L_):
    p = Prog()
    T = S
    ext = {}

    def E(name, shape, dt=F32):
        ext[name] = p.nc.dram_tensor(name, list(shape), dt, kind="ExternalInput").ap()
        return ext[name]

    x_in = E("x", [T, D])
    E("c", [128, 8])
    E("ada_w", [L, D, 6 * D])
    E("ada_b", [L, 128, 6 * D])
    E("ngb4", [L, 2, 128, D])
    E("ones32", [128, 128])
    E("posb", [128, T], I32)
    E("invf", [128, 2])
    E("ident", [128, 128])
    E("onesbf", [128, 64], BF16)
    E("identb", [128, 128], BF16)
    E("mla_w_in", [2, D, 672])
    E("qn", [2, 128, 3])
    E("kvn", [2, 128, 2])
    E("mla_w_uq", [2, 384, 1536])
    E("mla_w_ukv", [2, 256, 2048])
    E("mla_w_out", [2, 1024, D])
    E("ffn_wg", [2, 1, D, FF])
    E("ffn_wu", [2, 1, D, FF])
    E("ffn_wd", [2, 1, FF, D])
    E("ssm_w_in", [2, D, SSM_IN])
    E("cw", [2, 2, 128, 12, 4])
    E("cb", [2, 2, 128, 12])
    E("dtb", [2, 2, 64, 16])
    E("alog", [2, 2, 128, 16])
    E("dsk", [2, 2, 64, 16])
    E("sngb", [2, 2, 64, 1024])
    E("U", [64, 64])
    E("ones64", [64, 128])
    E("I64", [64, 64])
    E("NEGrep", [64, 512])
    E("ssm_w_out", [2, 2048, D])
    E("wr", [2, D, 8])
    E("moe_wg", [2, 8, D, FF])
    E("moe_wu", [2, 8, D, FF])
    E("moe_wd", [2, 8, FF, D])
    E("fgb", [128, D])
    out = p.nc.dram_tensor("out", [T, D], F32, kind="ExternalOutput").ap()

    sc = p.scratch
    modv = sc("modv", [L, 6, 128, D])
    cos2 = sc("cos2", [128, T])
    sin2s = sc("sin2s", [128, T])
    xa = sc("xa", [T, D])
    xb = sc("xb", [T, D])
    QN = sc("QN", [8, 128, T], BF16)
    QR = sc("QR", [4, 128, T], BF16)
    KN = sc("KN", [8, 128, T], BF16)
    KR = sc("KR", [32, T], BF16)
    V = sc("V", [T, D], BF16)
    OT8 = sc("OT8", [8, 128, T], BF16)
    OT16 = sc("OT16", [16, 128, T], BF16)
    PT24 = sc("PT24", [24, 128, T], BF16)
    ZT = sc("ZT", [T, 2048], BF16)
    DTt = sc("DTt", [T, 32])

    def fmv(l, i):
        return modv[l, i, 0, :].rearrange("(kc p) -> p kc", p=128)

    p.bind = {"c": ext["c"], "ada_w": ext["ada_w"], "ada_b": ext["ada_b"], "ngb": ext["ngb4"], "ones": ext["ones32"],
              "posb": ext["posb"], "invf": ext["invf"], "modv": modv, "cos2": cos2, "sin2s": sin2s}
    emit_mod(p, T, L)
    p.end_phase()

    xcur = x_in
    for l in range(L):
        j = l // 2
        if l % 2 == 0:
            p.bind = {"x": xcur, "a1": fmv(l, 0), "sh1": fmv(l, 1), "w_in": ext["mla_w_in"][j], "qn": ext["qn"][j],
                      "kvn": ext["kvn"][j], "w_uq": ext["mla_w_uq"][j], "w_ukv": ext["mla_w_ukv"][j], "cos2": cos2,
                      "sin2s": sin2s, "ident": ext["ident"], "QN": QN, "QR": QR, "KN": KN, "KR": KR, "V": V}
            emit_mla_pre(p, T)
            p.end_phase()
            p.bind = {"ones": ext["onesbf"], "OT": OT8}
            emit_attn(p, S, 16, src={"QN": QN, "QR": QR, "KN": KN, "KR": KR, "V": V})
            p.end_phase()
            KC, OT, w_out = 8, OT8, ext["mla_w_out"][j]
        else:
            p.bind = {"x": xcur, "a1": fmv(l, 0), "sh1": fmv(l, 1), "w_in": ext["ssm_w_in"][j], "ident": ext["ident"],
                      "PT24": PT24, "ZT": ZT, "DTt": DTt}
            emit_ssm_pre(p, T)
            p.end_phase()
            for gp in range(2):
                p.bind = {"PT24": PT24, "ZT": ZT, "DTt": DTt, "OTf": OT16, "cw": ext["cw"][j, gp], "cb": ext["cb"][j, gp],
                          "dtb": ext["dtb"][j, gp], "alog": ext["alog"][j, gp], "dsk": ext["dsk"][j, gp],
                          "ngb": ext["sngb"][j, gp], "U": ext["U"], "ones64": ext["ones64"], "I64": ext["I64"],
                          "NEGrep": ext["NEGrep"], "identb": ext["identb"]}
                emit_ssm_core(p, S, gp=gp)
                p.end_phase()
            KC, OT, w_out = 16, OT16, ext["ssm_w_out"][j]
        p.bind = {"x": xcur, "OT": OT, "w_out": w_out, "g1b": modv[l, 2], "y": xb}
        emit_outproj(p, T, KC)
        p.end_phase()
        final = (l == L - 1)
        p.bind = {"x": xb, "a2": fmv(l, 3), "sh2": fmv(l, 4), "g2b": modv[l, 5], "ident": ext["ident"],
                  "y": out if final else xa}
        if l % 2 == 0:
            p.bind.update({"wg": ext["ffn_wg"][j], "wu": ext["ffn_wu"][j], "wd": ext["ffn_wd"][j]})
            emit_ffn(p, T, 1, False, final)
        else:
            p.bind.update({"wg": ext["moe_wg"][j], "wu": ext["moe_wu"][j], "wd": ext["moe_wd"][j], "wr": ext["wr"][j],
                           "fgb": ext["fgb"]})
            emit_ffn(p, T, 8, True, final)
        xcur = xa
        if not final:
            p.end_phase()
    return p.finish()


def _fm(v):
    return np.ascontiguousarray(np.asarray(v).reshape(8, 128).T)


def _bc(v, n):
    v = np.asarray(v)
    return np.ascontiguousarray(np.broadcast_to(v, (n,) + v.shape))


def kernel(x, c, positions, ada_w, ada_b, norm_g,
           mla_w_in, mla_q_norm, mla_kv_norm, mla_w_uq, mla_w_ukv, mla_w_out,
           ssm_w_in, ssm_conv_w, ssm_conv_b, ssm_dt_bias, ssm_a_log, ssm_d, ssm_norm, ssm_w_out,
           ffn_w_gate, ffn_w_up, ffn_w_down,
           moe_w_router, moe_w_gate, moe_w_up, moe_w_down, final_norm):
    f32 = np.float32
    A = lambda a: np.ascontiguousarray(np.asarray(a))
    x = A(x).astype(f32, copy=False)
    pos = A(positions).astype(np.int32, copy=False)
    nc = build_fused(S_, L_)
    bf = mybir.dt.np_dtype(BF16) if hasattr(mybir.dt, "np_dtype") else None
    if bf is None:
        import ml_dtypes
        bf = ml_dtypes.bfloat16
    cwj, cbj = A(ssm_conv_w), A(ssm_conv_b)
    cw = np.empty((2, 2, 128, 12, 4), f32)
    cb = np.empty((2, 2, 128, 12), f32)
    for j in range(2):
        for gp in range(2):
            chs = list(range(gp * 8, gp * 8 + 8)) + [16 + 2 * gp, 17 + 2 * gp, 20 + 2 * gp, 21 + 2 * gp]
            cidx = np.array(chs)[:, None] * 128 + np.arange(128)[None, :]
            cw[j, gp] = cwj[j][:, cidx].transpose(2, 1, 0)
            cb[j, gp] = cbj[j][cidx].T
    hs = lambda v, n: np.ascontiguousarray(np.stack([np.stack([np.broadcast_to(A(v)[j][gp * 16:(gp + 1) * 16], (n, 16))
                                                                for gp in range(2)]) for j in range(2)]))
    sngb = np.ascontiguousarray(np.stack([np.stack([np.broadcast_to(A(ssm_norm)[j][gp * 1024:(gp + 1) * 1024], (64, 1024))
                                                    for gp in range(2)]) for j in range(2)]))
    shared = {
        "ada_w": A(ada_w), "ada_b": np.ascontiguousarray(np.broadcast_to(A(ada_b)[:, None, :], (L_, 128, 6 * D))),
        "ngb4": np.ascontiguousarray(np.broadcast_to(A(norm_g)[:, :, None, :], (L_, 2, 128, D))),
        "ones32": np.ones((128, 128), f32), "invf": rope_consts(), "ident": np.eye(128, dtype=f32),
        "onesbf": np.ones((128, 64), bf), "identb": np.eye(128).astype(bf),
        "mla_w_in": A(mla_w_in), "qn": np.ascontiguousarray(A(mla_q_norm).reshape(2, 3, 128).transpose(0, 2, 1)),
        "kvn": np.ascontiguousarray(A(mla_kv_norm).reshape(2, 2, 128).transpose(0, 2, 1)),
        "mla_w_uq": A(mla_w_uq), "mla_w_ukv": A(mla_w_ukv), "mla_w_out": A(mla_w_out),
        "ffn_wg": A(ffn_w_gate)[:, None], "ffn_wu": A(ffn_w_up)[:, None], "ffn_wd": A(ffn_w_down)[:, None],
        "ssm_w_in": A(ssm_w_in), "cw": cw, "cb": cb, "dtb": hs(ssm_dt_bias, 64), "alog": hs(ssm_a_log, 128),
        "dsk": hs(ssm_d, 64), "sngb": sngb, "ssm_w_out": A(ssm_w_out), "wr": A(moe_w_router),
        "moe_wg": A(moe_w_gate), "moe_wu": A(moe_w_up), "moe_wd": A(moe_w_down), "fgb": _bc(A(final_norm), 128),
    }
    shared.update(ssm_consts())
    maps = []
    for b in range(B_):
        m = dict(shared)
        m["x"] = np.ascontiguousarray(x[b])
        m["c"] = _fm(A(c)[b])
        m["posb"] = _bc(pos[b], 128)
        maps.append(m)
    res = run_bass_kernel_spmd(nc, maps, core_ids=list(range(B_)))
    return np.stack([np.asarray(res.results[b]["out"]) for b in range(B_)], axis=0).astype(f32, copy=False)
```
